# Optimizing a Trainium2 kernel written in Bass

```python
import jax, jax.numpy as jnp
from jax import lax
import numpy as np

D_MODEL = 4096
BATCH = 1
SEQ = 8192
DEPTH = 1

N_META = 16
NORM_EPS = 1e-6
DN_HEADS = 16
DN_DK = 128
DN_DV = 128
DN_CONV = 3
DN_CHUNK = 64
SWA_HQ = 16
SWA_HKV = 4
SWA_D = 128
SWA_WINDOW = 128
SWA_BLOCK = 128
N_EXPERTS = 16
EXPERT_FF = 2048
EC_CAPACITY = 2

DN_QK = DN_HEADS * DN_DK
DN_VW = DN_HEADS * DN_DV
DN_CONV_W = 2 * DN_QK + DN_VW
SWA_QW = SWA_HQ * SWA_D
SWA_KVW = SWA_HKV * SWA_D
IN_SPLITS = (DN_CONV_W, DN_VW, DN_HEADS, DN_HEADS, DN_HEADS, DN_HEADS, SWA_QW, SWA_KVW, SWA_KVW, D_MODEL, D_MODEL)
IN_WIDTH = sum(IN_SPLITS)

kernel_name = 'hybrid_deltanet_swa_ec_moe_encoder'


def _rmsnorm(x, w):
    xf = x.astype(jnp.float32)
    y = xf * lax.rsqrt(jnp.mean(xf * xf, axis=-1, keepdims=True) + NORM_EPS)
    return (y * w.astype(jnp.float32)).astype(x.dtype)


def _l2norm(x):
    xf = x.astype(jnp.float32)
    return xf * lax.rsqrt(jnp.sum(xf * xf, axis=-1, keepdims=True) + NORM_EPS)


def _centred_dwconv(x, w):
    K = w.shape[0]
    L = x.shape[1]
    p = K // 2
    xp = jnp.pad(x, ((0, 0), (p, K - 1 - p), (0, 0)))
    y = xp[:, :L] * w[0]
    for i in range(1, K):
        y = y + xp[:, i:i + L] * w[i]
    return y


def _chunk(x, pad_front):
    B, L = x.shape[:2]
    pad = (-L) % DN_CHUNK
    widths = [(0, 0)] * x.ndim
    widths[1] = (pad, 0) if pad_front else (0, pad)
    xp = jnp.pad(x, widths)
    n = xp.shape[1] // DN_CHUNK
    xp = xp.reshape((B, n, DN_CHUNK) + x.shape[2:])
    return jnp.moveaxis(xp, 3, 1)


def _unchunk(o, L, pad_front):
    B, H, N, C, E = o.shape
    o = jnp.moveaxis(o, 1, 3).reshape(B, N * C, H, E)
    pad = N * C - L
    return o[:, pad:] if pad_front else o[:, :L]


def _gated_delta_rule(q, k, v, beta, g):
    C = q.shape[-2]
    incl = jnp.tril(jnp.ones((C, C), dtype=bool))
    strict = jnp.tril(jnp.ones((C, C), dtype=bool), -1)
    gc = jnp.cumsum(g, axis=-1)
    decay = jnp.where(incl, jnp.exp(jnp.where(incl, gc[..., :, None] - gc[..., None, :], 0.0)), 0.0)
    kb = k * beta[..., None]
    m = jnp.where(strict, jnp.einsum('bhnid,bhnjd->bhnij', kb, k) * decay, 0.0)
    eye = jnp.eye(C, dtype=q.dtype)
    t = lax.linalg.triangular_solve(eye + m, jnp.broadcast_to(eye, m.shape), left_side=True, lower=True, unit_diagonal=True)
    u = jnp.einsum('bhnij,bhnje->bhnie', t, v * beta[..., None])
    w = jnp.einsum('bhnij,bhnjd->bhnid', t, kb * jnp.exp(gc)[..., None])
    qk = jnp.einsum('bhnid,bhnjd->bhnij', q, k) * decay
    q_dec = q * jnp.exp(gc)[..., None]
    g_last = gc[..., -1]
    k_dec = k * jnp.exp(g_last[..., None] - gc)[..., None]

    def step(s, xs):
        q_c, k_c, u_c, w_c, qk_c, gl = xs
        v_new = u_c - jnp.einsum('bhcd,bhde->bhce', w_c, s)
        o = jnp.einsum('bhcd,bhde->bhce', q_c, s) + jnp.einsum('bhij,bhje->bhie', qk_c, v_new)
        s = s * jnp.exp(gl)[..., None, None] + jnp.einsum('bhcd,bhce->bhde', k_c, v_new)
        return s, o

    B, H = q.shape[:2]
    s0 = jnp.zeros((B, H, q.shape[-1], v.shape[-1]), jnp.float32)
    xs = tuple(jnp.moveaxis(a, 2, 0) for a in (q_dec, k_dec, u, w, qk, g_last))
    _, o = lax.scan(step, s0, xs)
    return jnp.moveaxis(o, 0, 2)


def _deltanet_mixer(qkv, z, b_f, b_b, a_f, a_b, a_log_fwd, a_log_bwd, dt_bias_fwd, dt_bias_bwd, out_norm_w):
    B, L = qkv.shape[:2]
    q, k, v = jnp.split(qkv, [DN_QK, 2 * DN_QK], axis=-1)
    q = _l2norm(q.reshape(B, L, DN_HEADS, DN_DK)) * (DN_DK ** -0.5)
    k = _l2norm(k.reshape(B, L, DN_HEADS, DN_DK))
    v = v.reshape(B, L, DN_HEADS, DN_DV).astype(jnp.float32)

    def log_decay(a, a_log, dtb):
        return -jnp.exp(a_log.astype(jnp.float32)) * jax.nn.softplus(a.astype(jnp.float32) + dtb.astype(jnp.float32))

    g_f = log_decay(a_f, a_log_fwd, dt_bias_fwd)
    g_b = log_decay(a_b, a_log_bwd, dt_bias_bwd)
    beta_f = jax.nn.sigmoid(b_f.astype(jnp.float32))
    beta_b = jax.nn.sigmoid(b_b.astype(jnp.float32))

    o_f = _gated_delta_rule(*(_chunk(t, True) for t in (q, k, v, beta_f, g_f)))
    o_f = _unchunk(o_f, L, True)
    o_b = _gated_delta_rule(*(_chunk(jnp.flip(t, 1), False) for t in (q, k, v, beta_b, g_b)))
    o_b = jnp.flip(_unchunk(o_b, L, False), 1)
    o = o_f + o_b

    o = o * lax.rsqrt(jnp.mean(o * o, axis=-1, keepdims=True) + NORM_EPS) * out_norm_w.astype(jnp.float32)
    o = o * jax.nn.silu(z.reshape(B, L, DN_HEADS, DN_DV).astype(jnp.float32))
    return o.reshape(B, L, DN_VW).astype(z.dtype)


def _alibi_slopes(n):
    return 2.0 ** (-8.0 * jnp.arange(1, n + 1, dtype=jnp.float32) / n)


def _sink_probs(s, sink):
    m = jnp.maximum(jnp.max(s, axis=-1), sink)
    p = jnp.exp(s - m[..., None])
    return p / (jnp.sum(p, axis=-1, keepdims=True) + jnp.exp(sink - m)[..., None])


def _window_attention(q, k, v, attn_sink):
    B, L = q.shape[:2]
    S = L - N_META
    W = SWA_BLOCK
    NB = S // W
    G = SWA_HQ // SWA_HKV
    scale = SWA_D ** -0.5
    slopes = _alibi_slopes(SWA_HQ).reshape(SWA_HKV, G)
    sink = attn_sink.astype(jnp.float32).reshape(SWA_HKV, G)
    km, kr = k[:, :N_META], k[:, N_META:]
    vm, vr = v[:, :N_META], v[:, N_META:]

    def band(t):
        t = t.reshape(B, NB, W, SWA_HKV, SWA_D)
        t = jnp.pad(t, ((0, 0), (1, 1), (0, 0), (0, 0), (0, 0)))
        return jnp.concatenate([t[:, :-2], t[:, 1:-1], t[:, 2:]], axis=2)

    def with_meta(tm, tb):
        return jnp.concatenate([jnp.broadcast_to(tm[:, None], (B, NB, N_META, SWA_HKV, SWA_D)), tb], axis=2)

    kb = with_meta(km, band(kr))
    vb = with_meta(vm, band(vr))
    qr = q[:, N_META:].reshape(B, NB, W, SWA_HKV, G, SWA_D)
    s = jnp.einsum('bnqhgd,bnkhd->bhgnqk', qr, kb, preferred_element_type=jnp.float32) * scale

    dist = W + jnp.arange(W)[:, None] - jnp.arange(3 * W)[None, :]
    blk = jnp.arange(NB)[:, None]
    kblk = jnp.arange(3 * W)[None, :] // W
    blk_ok = ((kblk != 0) | (blk > 0)) & ((kblk != 2) | (blk < NB - 1))
    ok_band = (jnp.abs(dist) <= SWA_WINDOW)[None] & blk_ok[:, None, :]
    ok = jnp.concatenate([jnp.ones((NB, W, N_META), dtype=bool), ok_band], axis=-1)
    bias = jnp.concatenate([jnp.zeros((W, N_META), jnp.float32), -jnp.abs(dist).astype(jnp.float32)], axis=-1)
    s = jnp.where(ok, s + slopes[:, :, None, None, None] * bias, -jnp.inf)
    p = _sink_probs(s, sink[:, :, None, None])
    o_r = jnp.einsum('bhgnqk,bnkhd->bnqhgd', p.astype(v.dtype), vb).reshape(B, S, SWA_HQ, SWA_D)

    k_mq = jnp.concatenate([km, kr[:, :W]], axis=1)
    v_mq = jnp.concatenate([vm, vr[:, :W]], axis=1)
    qm = q[:, :N_META].reshape(B, N_META, SWA_HKV, G, SWA_D)
    s_m = jnp.einsum('bqhgd,bkhd->bhgqk', qm, k_mq, preferred_element_type=jnp.float32) * scale
    key_pos = jnp.arange(N_META + W)[None, :]
    q_pos = jnp.arange(N_META)[:, None]
    ok_m = (key_pos < N_META) | (key_pos - q_pos <= SWA_WINDOW)
    s_m = jnp.where(ok_m, s_m, -jnp.inf)
    p_m = _sink_probs(s_m, sink[:, :, None])
    o_m = jnp.einsum('bhgqk,bkhd->bqhgd', p_m.astype(v.dtype), v_mq).reshape(B, N_META, SWA_HQ, SWA_D)
    return jnp.concatenate([o_m, o_r], axis=1)


def _expert_choice_moe(h, w_router, w_gate, w_up, w_down):
    B, T, D = h.shape
    C = EC_CAPACITY * T // N_EXPERTS
    aff = jax.nn.softmax(jnp.einsum('btd,de->bte', h, w_router, preferred_element_type=jnp.float32), axis=-1)
    gates, idx = lax.top_k(jnp.swapaxes(aff, 1, 2), C)
    xe = jax.vmap(lambda hb, ib: hb[ib])(h, idx)
    hid = jax.nn.silu(jnp.einsum('becd,edf->becf', xe, w_gate)) * jnp.einsum('becd,edf->becf', xe, w_up)
    ye = jnp.einsum('becf,efd->becd', hid, w_down) * gates.astype(h.dtype)[..., None]
    return jax.vmap(lambda ib, yb: jnp.zeros((T, D), yb.dtype).at[ib.reshape(-1)].add(yb.reshape(-1, D)))(idx, ye)


def _layer(h, norm1_w, w_in, conv_w, a_log_fwd, a_log_bwd, dt_bias_fwd, dt_bias_bwd, out_norm_w,
           w_branch_a, attn_sink, w_branch_b, w_out, norm2_w, w_router, w_gate, w_up, w_down):
    B, L, _ = h.shape
    n = _rmsnorm(h, norm1_w)
    proj = n @ w_in
    offsets = [int(o) for o in np.cumsum(IN_SPLITS)[:-1]]
    qkv_a, z_a, b_f, b_b, a_f, a_b, q_b, k_b, v_b, gate_a, gate_b = jnp.split(proj, offsets, axis=-1)

    qkv_a = jax.nn.silu(_centred_dwconv(qkv_a, conv_w))
    o_a = _deltanet_mixer(qkv_a, z_a, b_f, b_b, a_f, a_b, a_log_fwd, a_log_bwd, dt_bias_fwd, dt_bias_bwd, out_norm_w)
    o_b = _window_attention(q_b.reshape(B, L, SWA_HQ, SWA_D), k_b.reshape(B, L, SWA_HKV, SWA_D),
                            v_b.reshape(B, L, SWA_HKV, SWA_D), attn_sink).reshape(B, L, SWA_QW)

    mixed = jax.nn.sigmoid(gate_a) * (o_a @ w_branch_a) + jax.nn.sigmoid(gate_b) * (o_b @ w_branch_b)
    h = h + mixed @ w_out
    h = h + _expert_choice_moe(_rmsnorm(h, norm2_w), w_router, w_gate, w_up, w_down)
    return h


def setup_inputs(seed: int = 0) -> dict:
    key = jax.random.key(seed)
    ks = jax.random.split(key, 24)
    f32 = jnp.float32
    nrm = lambda k, shape, s: jax.random.normal(k, shape, f32) * s
    dt = jnp.exp(jax.random.uniform(ks[6], (DEPTH, DN_HEADS), f32) * (jnp.log(0.1) - jnp.log(0.001)) + jnp.log(0.001))
    dt2 = jnp.exp(jax.random.uniform(ks[7], (DEPTH, DN_HEADS), f32) * (jnp.log(0.1) - jnp.log(0.001)) + jnp.log(0.001))
    return {
        'x': nrm(ks[0], (BATCH, SEQ, D_MODEL), 1.0),
        'meta_tokens': nrm(ks[1], (N_META, D_MODEL), 1.0),
        'norm1_w': 1.0 + nrm(ks[2], (DEPTH, D_MODEL), 0.02),
        'w_in': nrm(ks[3], (DEPTH, D_MODEL, IN_WIDTH), D_MODEL ** -0.5),
        'conv_w': nrm(ks[4], (DEPTH, DN_CONV, DN_CONV_W), DN_CONV ** -0.5),
        'a_log_fwd': jnp.log(jax.random.uniform(ks[5], (DEPTH, DN_HEADS), f32, 1.0, 16.0)),
        'a_log_bwd': jnp.log(jax.random.uniform(ks[8], (DEPTH, DN_HEADS), f32, 1.0, 16.0)),
        'dt_bias_fwd': dt + jnp.log(-jnp.expm1(-dt)),
        'dt_bias_bwd': dt2 + jnp.log(-jnp.expm1(-dt2)),
        'out_norm_w': 1.0 + nrm(ks[9], (DEPTH, DN_DV), 0.02),
        'w_branch_a': nrm(ks[10], (DEPTH, DN_VW, D_MODEL), DN_VW ** -0.5),
        'attn_sink': nrm(ks[11], (DEPTH, SWA_HQ), 0.5),
        'w_branch_b': nrm(ks[12], (DEPTH, SWA_QW, D_MODEL), SWA_QW ** -0.5),
        'w_out': nrm(ks[13], (DEPTH, D_MODEL, D_MODEL), D_MODEL ** -0.5),
        'norm2_w': 1.0 + nrm(ks[14], (DEPTH, D_MODEL), 0.02),
        'w_router': nrm(ks[15], (DEPTH, D_MODEL, N_EXPERTS), D_MODEL ** -0.5),
        'w_gate': nrm(ks[16], (DEPTH, N_EXPERTS, D_MODEL, EXPERT_FF), D_MODEL ** -0.5),
        'w_up': nrm(ks[17], (DEPTH, N_EXPERTS, D_MODEL, EXPERT_FF), D_MODEL ** -0.5),
        'w_down': nrm(ks[18], (DEPTH, N_EXPERTS, EXPERT_FF, D_MODEL), EXPERT_FF ** -0.5),
        'norm_f_w': 1.0 + nrm(ks[19], (D_MODEL,), 0.02),
    }


def reference(x, meta_tokens, norm1_w, w_in, conv_w, a_log_fwd, a_log_bwd, dt_bias_fwd, dt_bias_bwd,
              out_norm_w, w_branch_a, attn_sink, w_branch_b, w_out, norm2_w, w_router, w_gate, w_up,
              w_down, norm_f_w):
    B = x.shape[0]
    meta = jnp.broadcast_to(meta_tokens.astype(x.dtype)[None], (B, N_META, x.shape[-1]))
    h = jnp.concatenate([meta, x], axis=1)
    for i in range(DEPTH):
        h = _layer(h, norm1_w[i], w_in[i], conv_w[i], a_log_fwd[i], a_log_bwd[i], dt_bias_fwd[i],
                   dt_bias_bwd[i], out_norm_w[i], w_branch_a[i], attn_sink[i], w_branch_b[i], w_out[i],
                   norm2_w[i], w_router[i], w_gate[i], w_up[i], w_down[i])
    return _rmsnorm(h, norm_f_w)[:, N_META:]
```

```python
from contextlib import ExitStack
_UC = [0]
def _u(n):
    _UC[0] += 1
    return f'{n}_{_UC[0]}'
import numpy as np
import concourse.bass as bass
import concourse.mybir as mybir

F32 = mybir.dt.float32
BF16 = mybir.dt.bfloat16
I32 = mybir.dt.int32
U32 = mybir.dt.uint32
AF = mybir.ActivationFunctionType
ALU = mybir.AluOpType
AX = mybir.AxisListType

EPOCH = 8000
NDMA = 24


class Prog:
    def __init__(self, nc):
        self.nc = nc
        self.engs = {"pe": nc.tensor, "dve": nc.vector, "act": nc.scalar,
                     "pool": nc.gpsimd, "sp": nc.sync}
        self.sem = {}
        self.cnt = {}
        self.nsem = 0
        for e in self.engs:
            self._new_epoch(e)
        self.dsem = [nc.alloc_semaphore(name=f"dq{i}") for i in range(NDMA)]
        self.dcnt = [0] * NDMA
        self.drr = 0
        self.last_w = {}
        self.readers = {}
        self.seen = {e: {} for e in self.engs}
        self.semobj = {}
        self.ninst = 0
        self.kp = None
        self.shared = set()

    def _new_epoch(self, e):
        s = self.nc.alloc_semaphore(name=f"e_{e}_{self.nsem}")
        self.nsem += 1
        self.sem[e] = s
        self.cnt[e] = 0

    def _wait(self, e, tok):
        s, v = tok
        key = id(s)
        self.semobj[key] = s
        if self.seen[e].get(key, 0) >= v:
            return
        self.engs[e].wait_ge(s, v)
        self.seen[e][key] = v

    def _k(self, k):
        if self.kp is None:
            return k
        root = k[0] if isinstance(k, tuple) else k
        return k if root in self.shared else (self.kp, k)

    def _deps(self, r, w):
        r = [self._k(k) for k in r]
        w = [self._k(k) for k in w]
        toks = {}

        def add(tok):
            if tok is None:
                return
            k = id(tok[0])
            if k not in toks or toks[k][1] < tok[1]:
                toks[k] = tok

        for k in r:
            add(self.last_w.get(k))
        for k in w:
            add(self.last_w.get(k))
            for t in self.readers.get(k, {}).values():
                add(t)
        return list(toks.values())

    def _register(self, tok, r, w):
        r = [self._k(k) for k in r]
        w = [self._k(k) for k in w]
        for k in r:
            d = self.readers.setdefault(k, {})
            d[id(tok[0])] = tok
        for k in w:
            self.last_w[k] = tok
            self.readers[k] = {}

    def op(self, e, fn, r=(), w=()):
        for tok in self._deps(r, w):
            self._wait(e, tok)
        if self.cnt[e] >= EPOCH:
            self._new_epoch(e)
        inst = fn(self.engs[e])
        self.cnt[e] += 1
        inst.then_inc(self.sem[e], 1)
        tok = (self.sem[e], self.cnt[e])
        self._register(tok, r, w)
        self.ninst += 1
        return tok

    def group(self, e, fns, r=(), w=()):
        for tok in self._deps(r, w):
            self._wait(e, tok)
        if self.cnt[e] + len(fns) >= EPOCH:
            self._new_epoch(e)
        tok = None
        for fn in fns:
            inst = fn(self.engs[e])
            self.cnt[e] += 1
            inst.then_inc(self.sem[e], 1)
            tok = (self.sem[e], self.cnt[e])
            self.ninst += 1
        self._register(tok, r, w)
        return tok

    def dma(self, q, fn, r=(), w=(), inc=16):
        i = self.drr
        self.drr = (i + 1) % NDMA
        if self.dcnt[i] > 0:
            self._wait(q, (self.dsem[i], self.dcnt[i]))
        for tok in self._deps(r, w):
            self._wait(q, tok)
        inst = fn(self.engs[q])
        self.dcnt[i] += inc
        inst.then_inc(self.dsem[i], inc)
        tok = (self.dsem[i], self.dcnt[i])
        self._register(tok, r, w)
        self.ninst += 1
        return tok

    def wait_all(self, e, keys):
        for k in keys:
            t = self.last_w.get(k)
            if t is not None:
                self._wait(e, t)

    def barrier(self):
        toks = [(self.sem[f], self.cnt[f]) for f in self.engs if self.cnt[f] > 0]
        toks += [(self.dsem[i], self.dcnt[i]) for i in range(NDMA) if self.dcnt[i] > 0]
        for e in self.engs:
            for tok in toks:
                self._wait(e, tok)
NT = 65
NR = NT * 128
CW = 1544
EPS = 1e-6


def mm(ps, lhsT, rhs, start=True, stop=True):
    return lambda e: e.matmul(ps, lhsT=lhsT, rhs=rhs, start=start, stop=stop)


def phase_A(P, nc, D, C, nt=NT):
    with ExitStack() as ES:
        W = ES.enter_context(nc.sbuf_tensor(_u("W"), [128, 32, CW], BF16))
        xt0 = ES.enter_context(nc.sbuf_tensor(_u("xt0"), [128, 4096], F32))
        xt1 = ES.enter_context(nc.sbuf_tensor(_u("xt1"), [128, 4096], F32))
        xn = ES.enter_context(nc.sbuf_tensor(_u("xn"), [128, 4096], BF16))
        xT = ES.enter_context(nc.sbuf_tensor(_u("xT"), [128, 32, 128], BF16))
        proj = ES.enter_context(nc.sbuf_tensor(_u("proj"), [128, CW], F32))
        rr = ES.enter_context(nc.sbuf_tensor(_u("rr"), [128, 9, 768], BF16))
        cw = ES.enter_context(nc.sbuf_tensor(_u("cw"), [128, 3, 768], F32))
        qs = ES.enter_context(nc.sbuf_tensor(_u("qs"), [128, 768], F32))
        qkvb = ES.enter_context(nc.sbuf_tensor(_u("qkvb"), [128, 768], BF16))
        zsb = ES.enter_context(nc.sbuf_tensor(_u("zsb"), [128, 256], BF16))
        swb = ES.enter_context(nc.sbuf_tensor(_u("swb"), [128, 512], BF16))
        junk = ES.enter_context(nc.sbuf_tensor(_u("junk"), [128, 128], BF16))
        sm = ES.enter_context(nc.sbuf_tensor(_u("sm"), [128, 64], F32))
        w1c = ES.enter_context(nc.sbuf_tensor(_u("w1c"), [128, 32], F32))
        dnp = ES.enter_context(nc.sbuf_tensor(_u("dnp"), [128, 8], F32))
        shm = ES.enter_context(nc.sbuf_tensor(_u("shm"), [128, 5, 128], BF16))
        tp0 = ES.enter_context(nc.psum_tensor(_u("tp0"), [128, 512], BF16))
        tp1 = ES.enter_context(nc.psum_tensor(_u("tp1"), [128, 512], BF16))
        pj0 = ES.enter_context(nc.psum_tensor(_u("pj0"), [128, 512], F32))
        pj1 = ES.enter_context(nc.psum_tensor(_u("pj1"), [128, 512], F32))
        pj2 = ES.enter_context(nc.psum_tensor(_u("pj2"), [128, 512], F32))
        pj3 = ES.enter_context(nc.psum_tensor(_u("pj3"), [128, 512], F32))
        cv0 = ES.enter_context(nc.psum_tensor(_u("cv0"), [128, 512], F32))
        cv1 = ES.enter_context(nc.psum_tensor(_u("cv1"), [128, 512], F32))
        xt = [xt0, xt1]
        tp = [tp0, tp1]
        pj = [pj0, pj1, pj2, pj3]
        P.dma("sp", lambda q: q.dma_start(out=w1c[:], in_=D["w1c"]), w=["w1c"])
        P.dma("sp", lambda q: q.dma_start(out=dnp[:], in_=D["dnp"]), w=["dnp"])
        P.dma("sp", lambda q: q.dma_start(out=cw[:], in_=D["cwbc"]), w=["cw"])
        P.dma("sp", lambda q: q.dma_start(out=shm[:], in_=D["shm"]), w=["shm"])
        P.op("act", lambda e: e.activation(out=sm[:, 16:20], in_=dnp[:, 0:4], func=AF.Exp), r=["dnp"], w=["negA"])
        P.op("dve", lambda e: e.tensor_scalar(out=sm[:, 16:20], in0=sm[:, 16:20], scalar1=-1.0, scalar2=None, op0=ALU.mult), r=["negA"], w=["negA"])
        for kc in range(32):
            b = kc % 2
            P.dma("sp", lambda q, kc=kc, b=b: q.dma_start(out=xt[b][:, 0:CW], in_=D["w_in"][kc * 128:(kc + 1) * 128, :]), w=[("xt", b)])
            P.op("act", lambda e, kc=kc, b=b: e.activation(out=W[:, kc, :], in_=xt[b][:, 0:CW], func=AF.Copy, scale=w1c[:, kc:kc + 1]),
                 r=[("xt", b), "w1c"], w=["W"])
        ident = C["identb"]

        def conv_tile(t):
            s, sp_, sn = (t % 3) * 3, ((t - 1) % 3) * 3, ((t + 1) % 3) * 3
            for half, (c0, c1, cv) in enumerate([(0, 512, cv0), (512, 768, cv1)]):
                fns = []
                ops = [(shm[:, 0, :], rr[:, s + 0, c0:c1]), (ident[:], rr[:, s + 1, c0:c1]), (shm[:, 1, :], rr[:, s + 2, c0:c1])]
                if t > 0:
                    ops.append((shm[:, 2, :], rr[:, sp_ + 0, c0:c1]))
                if t < nt - 1:
                    ops.append((shm[:, 3, :], rr[:, sn + 2, c0:c1]))
                for i, (l, r_) in enumerate(ops):
                    fns.append(mm(cv[:, 0:c1 - c0], l, r_, start=(i == 0), stop=(i == len(ops) - 1)))
                P.group("pe", fns, r=[("rr", t % 3), ("rr", (t - 1) % 3), ("rr", (t + 1) % 3), "shm", "identb"], w=[("cv", half)])
                P.op("act", lambda e, c0=c0, c1=c1, cv=cv: e.activation(out=qs[:, c0:c1], in_=cv[:, 0:c1 - c0], func=AF.Silu),
                     r=[("cv", half)], w=[("qs", half)])
            for i in range(4):
                P.op("act", lambda e, i=i: e.activation(out=junk[:], in_=qs[:, i * 128:(i + 1) * 128], func=AF.Square, accum_out=sm[:, 4 + i:5 + i]),
                     r=[("qs", 0), ("qs", 1)], w=["junk", "ss4"])
            P.op("act", lambda e: e.activation(out=sm[:, 8:12], in_=sm[:, 4:8], func=AF.Sqrt, bias=EPS, scale=1.0), r=["ss4"], w=["rt4"])
            P.op("dve", lambda e: e.reciprocal(out=sm[:, 12:16], in_=sm[:, 8:12]), r=["rt4"], w=["r4"])
            P.op("dve", lambda e: e.tensor_scalar(out=sm[:, 12:14], in0=sm[:, 12:14], scalar1=float(128 ** -0.5), scalar2=None, op0=ALU.mult), r=["r4"], w=["r4"])
            for i in range(4):
                P.op("dve", lambda e, i=i: e.tensor_scalar(out=qkvb[:, i * 128:(i + 1) * 128], in0=qs[:, i * 128:(i + 1) * 128],
                                                          scalar1=sm[:, 12 + i:13 + i], scalar2=None, op0=ALU.mult),
                     r=[("qs", 0), ("qs", 1), "r4"], w=["qkvb"])
            P.op("pool", lambda e: e.tensor_copy(out=qkvb[:, 512:768], in_=qs[:, 512:768]), r=[("qs", 0), ("qs", 1)], w=["qkvb"])
            if t == 0:
                P.op("pool", lambda e: e.memset(qkvb[0:112, :], 0.0), r=["qkvb"], w=["qkvb"])
            P.dma("sp", lambda q, t=t: q.dma_start(out=D["dn_qkv"][t * 128:(t + 1) * 128, :], in_=qkvb[:]), r=["qkvb"], w=["dn_qkv"])

        for t in range(nt):
            b = t % 2
            P.dma("sp", lambda q, t=t, b=b: q.dma_start(out=xt[b][:], in_=D["xp"][t * 128:(t + 1) * 128, :]), w=[("xt", b)])
            P.op("act", lambda e, b=b: e.activation(out=xn[:], in_=xt[b][:], func=AF.Square, accum_out=sm[:, 0:1]), r=[("xt", b)], w=["xn", "ss"])
            P.op("act", lambda e: e.activation(out=sm[:, 1:2], in_=sm[:, 0:1], func=AF.Sqrt, bias=EPS, scale=1.0 / 4096), r=["ss"], w=["rt"])
            P.op("dve", lambda e: e.reciprocal(out=sm[:, 2:3], in_=sm[:, 1:2]), r=["rt"], w=["rstd"])
            P.op("act", lambda e, b=b: e.activation(out=xn[:], in_=xt[b][:], func=AF.Copy, scale=sm[:, 2:3]), r=[("xt", b), "rstd"], w=["xn"])
            for g in range(8):
                tb = g % 2
                P.group("pe", [(lambda e, g=g, j=j, tb=tb: e.transpose(tp[tb][:, j * 128:(j + 1) * 128], xn[:, (4 * g + j) * 128:(4 * g + j + 1) * 128], ident[:]))
                               for j in range(4)], r=["xn", "identb"], w=[("tp", tb)])
                eng = "act" if g % 2 == 0 else "dve"
                if eng == "act":
                    P.op("act", lambda e, g=g, tb=tb: e.copy(out=xT[:, 4 * g:4 * g + 4, :], in_=tp[tb][:].rearrange("p (a b) -> p a b", b=128)), r=[("tp", tb)], w=["xT"])
                else:
                    P.op("dve", lambda e, g=g, tb=tb: e.tensor_copy(out=xT[:, 4 * g:4 * g + 4, :], in_=tp[tb][:].rearrange("p (a b) -> p a b", b=128)), r=[("tp", tb)], w=["xT"])
            for cg in range(4):
                c0 = cg * 512
                c1 = min(CW, c0 + 512)
                P.group("pe", [mm(pj[cg][:, 0:c1 - c0], xT[:, kc, :], W[:, kc, c0:c1], start=(kc == 0), stop=(kc == 31)) for kc in range(32)],
                        r=["xT", "W"], w=[("pj", cg)])
                P.op("act" if cg % 2 == 0 else "dve",
                     (lambda e, cg=cg, c0=c0, c1=c1: e.copy(out=proj[:, c0:c1], in_=pj[cg][:, 0:c1 - c0])) if cg % 2 == 0 else
                     (lambda e, cg=cg, c0=c0, c1=c1: e.tensor_copy(out=proj[:, c0:c1], in_=pj[cg][:, 0:c1 - c0])),
                     r=[("pj", cg)], w=["proj"])
            s = (t % 3) * 3
            for i in range(3):
                P.op("pool", lambda e, i=i, s=s: e.tensor_tensor(out=rr[:, s + i, :], in0=proj[:, 0:768], in1=cw[:, i, :], op=ALU.mult),
                     r=["proj", "cw"], w=[("rr", t % 3)])
            P.op("act", lambda e: e.activation(out=zsb[:], in_=proj[:, 768:1024], func=AF.Silu), r=["proj"], w=["zsb"])
            P.dma("sp", lambda q, t=t: q.dma_start(out=D["dn_z"][t * 128:(t + 1) * 128, :], in_=zsb[:]), r=["zsb"], w=["dn_z"])
            P.op("act", lambda e: e.activation(out=sm[:, 40:44], in_=proj[:, 1536:1540], func=AF.Sigmoid), r=["proj"], w=["sc"])
            P.op("dve", lambda e: e.tensor_tensor(out=sm[:, 20:24], in0=proj[:, 1540:1544], in1=dnp[:, 4:8], op=ALU.add), r=["proj", "dnp"], w=["spx"])
            P.op("act", lambda e: e.activation(out=sm[:, 24:28], in_=sm[:, 20:24], func=AF.Abs), r=["spx"], w=["spa"])
            P.op("act", lambda e: e.activation(out=sm[:, 28:32], in_=sm[:, 24:28], func=AF.Exp, scale=-1.0), r=["spa"], w=["spe"])
            P.op("act", lambda e: e.activation(out=sm[:, 32:36], in_=sm[:, 28:32], func=AF.Ln, bias=1.0, scale=1.0), r=["spe"], w=["spl"])
            P.op("dve", lambda e: e.tensor_scalar(out=sm[:, 36:40], in0=sm[:, 20:24], scalar1=0.0, scalar2=None, op0=ALU.max), r=["spx"], w=["spm"])
            P.op("dve", lambda e: e.tensor_tensor(out=sm[:, 36:40], in0=sm[:, 36:40], in1=sm[:, 32:36], op=ALU.add), r=["spm", "spl"], w=["spm"])
            P.op("dve", lambda e: e.tensor_tensor(out=sm[:, 44:48], in0=sm[:, 36:40], in1=sm[:, 16:20], op=ALU.mult), r=["spm", "negA"], w=["sc"])
            P.dma("sp", lambda q, t=t: q.dma_start(out=D["dn_sc"][t * 128:(t + 1) * 128, :], in_=sm[:, 40:48]), r=["sc"], w=["dn_sc"])
            P.op("pool", lambda e: e.tensor_copy(out=swb[:], in_=proj[:, 1024:1536]), r=["proj"], w=["swb"])
            P.dma("sp", lambda q, t=t: q.dma_start(out=D["sw_qkv"][t * 128:(t + 1) * 128, :], in_=swb[:]), r=["swb"], w=["sw_qkv"])
            if t > 0:
                conv_tile(t - 1)
        conv_tile(nt - 1)
def dn_pass(P, nc, D, C, d, nt=NT):
    with ExitStack() as ES:
        qk0 = ES.enter_context(nc.sbuf_tensor(_u("qk0"), [128, 768], BF16))
        qk1 = ES.enter_context(nc.sbuf_tensor(_u("qk1"), [128, 768], BF16))
        sc0 = ES.enter_context(nc.sbuf_tensor(_u("sc0"), [128, 8], F32))
        sc1 = ES.enter_context(nc.sbuf_tensor(_u("sc1"), [128, 8], F32))
        kqT_ = ES.enter_context(nc.sbuf_tensor(_u("kqT"), [128, 2, 2, 128], BF16))
        fa_ = ES.enter_context(nc.sbuf_tensor(_u("f32a"), [128, 2, 8, 128], F32))
        ba_ = ES.enter_context(nc.sbuf_tensor(_u("bfa"), [128, 2, 11, 128], BF16))
        Sb = ES.enter_context(nc.sbuf_tensor(_u("Sb"), [128, 2, 128], BF16))
        ds_ = ES.enter_context(nc.sbuf_tensor(_u("dsm"), [128, 2, 32], F32))
        osb_ = ES.enter_context(nc.sbuf_tensor(_u("osb"), [64, 2, 2, 128], F32))
        ofl_ = ES.enter_context(nc.sbuf_tensor(_u("ofl"), [64, 2, 2, 128], F32))
        zl_ = ES.enter_context(nc.sbuf_tensor(_u("zl"), [64, 2, 2, 128], BF16))
        oout_ = ES.enter_context(nc.sbuf_tensor(_u("oout"), [64, 2, 2, 128], BF16))
        msk = ES.enter_context(nc.sbuf_tensor(_u("msk"), [128, 3, 128], F32))
        lsel = ES.enter_context(nc.sbuf_tensor(_u("lsel"), [128, 4], F32))
        onw = ES.enter_context(nc.sbuf_tensor(_u("onw"), [128, 128], F32))
        xa0 = ES.enter_context(nc.psum_tensor(_u("xa0"), [128, 512], F32))
        xb0 = ES.enter_context(nc.psum_tensor(_u("xb0"), [128, 512], F32))
        xc0 = ES.enter_context(nc.psum_tensor(_u("xc0"), [128, 512], F32))
        xa1 = ES.enter_context(nc.psum_tensor(_u("xa1"), [128, 512], F32))
        xb1 = ES.enter_context(nc.psum_tensor(_u("xb1"), [128, 512], F32))
        xc1 = ES.enter_context(nc.psum_tensor(_u("xc1"), [128, 512], F32))
        xd0 = ES.enter_context(nc.psum_tensor(_u("xd0"), [128, 512], F32))
        xd1 = ES.enter_context(nc.psum_tensor(_u("xd1"), [128, 512], F32))
        qk = [qk0, qk1]
        scs = [sc0, sc1]
        XA, XB, XC, XD = [xa0, xa1], [xb0, xb1], [xc0, xc1], [xd0, xd1]
        identb, identf, onesf = C["identb"], C["identf"], C["onesf"]
        P.shared = {"qk", "sc", "identb", "identf", "onesf", "msk", "lsel", "onw", "dn_qkv", "dn_sc", "dn_z", "o_f", "oab", "S"}
        P.dma("sp", lambda q: q.dma_start(out=msk[:], in_=D["dnmask"][d]), w=["msk"])
        P.dma("sp", lambda q: q.dma_start(out=lsel[:], in_=D["lastsel"][d]), w=["lsel"])
        P.dma("sp", lambda q: q.dma_start(out=onw[:], in_=D["onwbc"]), w=["onw"])
        for h in range(2):
            P.op("pool", lambda e, h=h: e.memset(Sb[:, h, :], 0.0), w=[("S", h)])

        def unit(h, t, b):
            kqT, fa, ba, ds = kqT_[:, h], fa_[:, h], ba_[:, h], ds_[:, h]
            osb, ofl, zl, oout = osb_[:, h], ofl_[:, h], zl_[:, h], oout_[:, h]
            pA = XA[h]
            pB, pD = XB[h][:, 0:128], XB[h][:, 128:256]
            pC, pE = XC[h][:, 0:128], XC[h][:, 128:256]
            pF, pG = XD[h][:, 0:128], XD[h][:, 128:256]
            pT = XA[h][:, 256:384].bitcast(BF16)
            qn = qk[b][:, h * 128:(h + 1) * 128]
            kn = qk[b][:, 256 + h * 128:256 + (h + 1) * 128]
            vv = qk[b][:, 512 + h * 128:512 + (h + 1) * 128]
            beta = scs[b][:, d * 2 + h:d * 2 + h + 1]
            g = scs[b][:, 4 + d * 2 + h:4 + d * 2 + h + 1]
            rq = [("qk", b), ("sc", b)]
            yield P.group("pe", [lambda e: e.transpose(pT[:, 0:128], kn, identb[:]), lambda e: e.transpose(pT[:, 128:256], qn, identb[:])],
                          r=rq + ["identb"], w=["pT"])
            yield P.op("act", lambda e: e.copy(out=kqT, in_=pT[:, 0:256].rearrange("p (a b) -> p a b", b=128)), r=["pT"], w=["kqT"])
            yield P.op("dve", lambda e: e.tensor_scalar(out=fa[:, 0, :], in0=onesf[:], scalar1=g, scalar2=None, op0=ALU.mult), r=rq + ["onesf"], w=["gB"])
            yield P.group("pe", [mm(pA[:, 0:128], fa[:, 0, :], msk[:, 0, :]), mm(pA[:, 128:129], msk[:, 0, :], g)], r=["gB", "msk", "kqT"] + rq, w=["pA"])
            yield P.op("act", lambda e: e.copy(out=ds[:, 0:1], in_=pA[:, 128:129]), r=["pA"], w=["gc"])
            yield P.op("dve", lambda e: e.tensor_scalar(out=fa[:, 1, :], in0=pA[:, 0:128], scalar1=ds[:, 0:1], scalar2=0.0, op0=ALU.subtract, op1=ALU.max), r=["pA", "gc"], w=["dec"])
            yield P.op("dve", lambda e: e.tensor_scalar(out=fa[:, 2, :], in0=pA[:, 0:128], scalar1=ds[:, 0:1], scalar2=0.0, op0=ALU.subtract, op1=ALU.min), r=["pA", "gc"], w=["decT"])
            yield P.op("act", lambda e: e.activation(out=fa[:, 1, :], in_=fa[:, 1, :], func=AF.Exp, scale=-1.0), r=["dec"], w=["dec"])
            yield P.op("act", lambda e: e.activation(out=fa[:, 2, :], in_=fa[:, 2, :], func=AF.Exp), r=["decT"], w=["decT"])
            yield P.op("dve", lambda e: e.tensor_tensor(out=fa[:, 1, :], in0=fa[:, 1, :], in1=msk[:, 1, :], op=ALU.mult), r=["dec", "msk"], w=["dec"])
            yield P.op("pool", lambda e: e.tensor_tensor(out=fa[:, 2, :], in0=fa[:, 2, :], in1=msk[:, 2, :], op=ALU.mult), r=["decT", "msk"], w=["decT"])
            yield P.op("pe", mm(pB, kqT[:, 0, :], kqT[:, 0, :]), r=["kqT"], w=["pB"])
            yield P.op("pe", mm(pC, kqT[:, 0, :], kqT[:, 1, :]), r=["kqT"], w=["pC"])
            yield P.op("dve", lambda e: e.tensor_scalar(out=ds[:, 1:2], in0=beta, scalar1=-1.0, scalar2=None, op0=ALU.mult), r=rq, w=["nbeta"])
            yield P.op("dve", lambda e: e.scalar_tensor_tensor(out=fa[:, 3, :], in0=pB, scalar=ds[:, 1:2], in1=fa[:, 1, :], op0=ALU.mult, op1=ALU.mult),
                       r=["pB", "nbeta", "dec"], w=["R"])
            yield P.op("dve", lambda e: e.tensor_tensor(out=ba[:, 0, :], in0=pC, in1=fa[:, 2, :], op=ALU.mult), r=["pC", "decT"], w=["AT"])
            yield P.op("pe", lambda e: e.transpose(pD, fa[:, 3, :], identf[:]), r=["R", "identf"], w=["pD"])
            yield P.op("act", lambda e: e.copy(out=fa[:, 4, :], in_=pD), r=["pD"], w=["Pm"])
            yield P.op("dve", lambda e: e.tensor_tensor(out=fa[:, 5, :], in0=fa[:, 4, :], in1=identf[:], op=ALU.add), r=["Pm", "identf"], w=["TT"])
            Rk, Pk = 3, 4
            for k in range(1, 6):
                Rn, Pn = (6, 7) if Rk == 3 else (3, 4)
                yield P.op("pe", mm(pD, fa[:, Pk, :], fa[:, Rk, :]), r=["R", "Pm", "R2", "P2"], w=["pD"])
                yield P.op("act", lambda e: e.copy(out=fa[:, Rn, :], in_=pD), r=["pD"], w=["R2" if Rn == 6 else "R"])
                if k < 5:
                    yield P.op("pe", mm(pE, fa[:, Rk, :], fa[:, Pk, :]), r=["R", "Pm", "R2", "P2", "AT"], w=["pE"])
                    yield P.op("dve", lambda e: e.tensor_copy(out=fa[:, Pn, :], in_=pE), r=["pE"], w=["P2" if Pn == 7 else "Pm"])
                yield P.op("pe", mm(pF, fa[:, Rn, :], fa[:, 5, :]), r=["R", "R2", "TT"], w=["pF"])
                yield P.op("dve", lambda e: e.tensor_tensor(out=fa[:, 5, :], in0=fa[:, 5, :], in1=pF, op=ALU.add), r=["pF", "TT"], w=["TT"])
                Rk, Pk = Rn, Pn
            yield P.op("act", lambda e: e.copy(out=ba[:, 1, :], in_=fa[:, 5, :]), r=["TT"], w=["TTb"])
            yield P.op("act", lambda e: e.activation(out=ds[:, 2:3], in_=ds[:, 0:1], func=AF.Exp), r=["gc"], w=["egc"])
            yield P.op("dve", lambda e: e.tensor_tensor(out=ds[:, 3:4], in0=ds[:, 2:3], in1=beta, op=ALU.mult), r=["egc"] + rq, w=["bk"])
            yield P.op("act", lambda e: e.activation(out=ba[:, 2, :], in_=vv, func=AF.Copy, scale=beta), r=rq, w=["vb"])
            yield P.op("act", lambda e: e.activation(out=ba[:, 3, :], in_=kn, func=AF.Copy, scale=ds[:, 3:4]), r=rq + ["bk"], w=["kbg"])
            yield P.op("pe", mm(pB, ba[:, 1, :], ba[:, 2, :]), r=["TTb", "vb"], w=["pB"])
            yield P.op("pe", mm(pC, ba[:, 1, :], ba[:, 3, :]), r=["TTb", "kbg"], w=["pC"])
            yield P.op("act", lambda e: e.copy(out=ba[:, 4, :], in_=pB), r=["pB"], w=["u"])
            yield P.op("act", lambda e: e.activation(out=ba[:, 5, :], in_=pC, func=AF.Copy, scale=-1.0), r=["pC"], w=["wneg"])
            yield P.op("dve", lambda e: e.tensor_scalar(out=ds[:, 4:6], in0=lsel[:, 0:2], scalar1=ds[:, 0:1], scalar2=None, op0=ALU.mult), r=["lsel", "gc"], w=["gsel"])
            yield P.op("pe", mm(pA[:, 0:2], onesf[:], ds[:, 4:6]), r=["onesf", "gsel"], w=["pA"])
            yield P.op("act", lambda e: e.copy(out=ds[:, 6:8], in_=pA[:, 0:2]), r=["pA"], w=["glb"])
            yield P.op("act", lambda e: e.activation(out=ds[:, 8:10], in_=ds[:, 6:8], func=AF.Exp), r=["glb"], w=["egl"])
            yield P.op("dve", lambda e: e.tensor_copy(out=ds[0:64, 10:11], in_=ds[0:64, 6:7]), r=["glb"], w=["glc"])
            yield P.op("dve", lambda e: e.tensor_copy(out=ds[64:128, 10:11], in_=ds[64:128, 7:8]), r=["glb"], w=["glc"])
            yield P.op("act", lambda e: e.activation(out=ds[:, 11:12], in_=ds[:, 0:1], func=AF.Exp, scale=-1.0, bias=ds[:, 10:11]), r=["gc", "glc"], w=["kds"])
            yield P.op("dve", lambda e: e.tensor_scalar(out=ds[:, 20:22], in0=lsel[:, 2:4], scalar1=ds[:, 11:12], scalar2=None, op0=ALU.mult), r=["lsel", "kds"], w=["kds2"])
            yield P.op("dve", lambda e: e.tensor_scalar(out=ba[:, 6, :], in0=kn, scalar1=ds[:, 20:21], scalar2=None, op0=ALU.mult), r=rq + ["kds2"], w=["kdec"])
            yield P.op("pool", lambda e: e.tensor_scalar(out=ba[:, 10, :], in0=kn, scalar1=ds[:, 21:22], scalar2=None, op0=ALU.mult), r=rq + ["kds2"], w=["kdec"])
            yield P.op("act", lambda e: e.activation(out=ba[:, 7, :], in_=identb[:], func=AF.Copy, scale=ds[:, 2:3]), r=["identb", "egc"], w=["Dg"])
            yield P.group("pe", [mm(pD, qn, ba[:, 7, :], True, False), mm(pD, ba[:, 5, :], ba[:, 0, :], False, True)],
                          r=rq + ["Dg", "wneg", "AT"], w=["pD"])
            yield P.op("act", lambda e: e.copy(out=ba[:, 8, :], in_=pD), r=["pD"], w=["QeffT"])
            for c in ([0, 1] if d == 0 else [1, 0]):
                cs = slice(c * 64, (c + 1) * 64)
                kd = 6 if c == 0 else 10
                yield P.op("pe", mm(pE, ba[:, 5, :], ba[:, kd, :]), r=["wneg", "kdec"], w=["pE"])
                yield P.op("dve", lambda e: e.scalar_tensor_tensor(out=ba[:, 9, :], in0=identf[:], scalar=ds[:, 8 + c:9 + c], in1=pE, op0=ALU.mult, op1=ALU.add),
                           r=["pE", "egl", "identf"], w=["TrT"])
                yield P.group("pe", [mm(pF[0:64, :], ba[:, 0, cs], ba[:, 4, :], True, False), mm(pF[0:64, :], ba[:, 8, cs], Sb[:, h, :], False, True)],
                              r=["AT", "u", "QeffT", ("S", h)], w=["pF"])
                yield P.group("pe", [mm(pG, ba[:, kd, :], ba[:, 4, :], True, False), mm(pG, ba[:, 9, :], Sb[:, h, :], False, True)],
                              r=["kdec", "u", "TrT", ("S", h)], w=["pG"])
                yield P.op("act", lambda e: e.copy(out=osb[:, c, :], in_=pF[0:64, :]), r=["pF", "pG"], w=[("osb", c)])
                yield P.op("act", lambda e: e.copy(out=Sb[:, h, :], in_=pG), r=["pG"], w=[("S", h)])
                r0 = t * 128 + c * 64
                if d == 0:
                    yield P.dma("sp", lambda q: q.dma_start(out=D["o_f"][r0:r0 + 64, h * 128:(h + 1) * 128], in_=osb[:, c, :]), r=[("osb", c)], w=["o_f"])
                else:
                    yield P.dma("sp", lambda q: q.dma_start(out=ofl[:, c, :], in_=D["o_f"][r0:r0 + 64, h * 128:(h + 1) * 128]), r=["o_f"], w=[("ofl", c)])
                    yield P.dma("sp", lambda q: q.dma_start(out=zl[:, c, :], in_=D["dn_z"][r0:r0 + 64, h * 128:(h + 1) * 128]), r=["dn_z"], w=[("zl", c)])
                    yield P.op("dve", lambda e: e.tensor_tensor(out=ofl[:, c, :], in0=ofl[:, c, :], in1=osb[:, c, :], op=ALU.add), r=[("ofl", c), ("osb", c)], w=[("ofl", c)])
                    yield P.op("act", lambda e: e.activation(out=osb[:, c, :], in_=ofl[:, c, :], func=AF.Square, accum_out=ds[0:64, 12 + c:13 + c]), r=[("ofl", c)], w=[("osb", c), ("oss", c)])
                    yield P.op("act", lambda e: e.activation(out=ds[0:64, 14 + c:15 + c], in_=ds[0:64, 12 + c:13 + c], func=AF.Sqrt, bias=EPS, scale=1.0 / 128), r=[("oss", c)], w=[("ort", c)])
                    yield P.op("dve", lambda e: e.reciprocal(out=ds[0:64, 16 + c:17 + c], in_=ds[0:64, 14 + c:15 + c]), r=[("ort", c)], w=[("orr", c)])
                    yield P.op("dve", lambda e: e.scalar_tensor_tensor(out=ofl[:, c, :], in0=ofl[:, c, :], scalar=ds[0:64, 16 + c:17 + c], in1=onw[0:64, :], op0=ALU.mult, op1=ALU.mult),
                               r=[("ofl", c), ("orr", c), "onw"], w=[("ofl", c)])
                    yield P.op("dve", lambda e: e.tensor_tensor(out=oout[:, c, :], in0=ofl[:, c, :], in1=zl[:, c, :], op=ALU.mult), r=[("ofl", c), ("zl", c)], w=[("oout", c)])
                    yield P.dma("sp", lambda q: q.dma_start(out=D["oab"][r0:r0 + 64, h * 128:(h + 1) * 128], in_=oout[:, c, :]), r=[("oout", c)], w=["oab"])

        order = list(range(nt)) if d == 0 else list(range(nt - 1, -1, -1))
        for it, t in enumerate(order):
            b = it % 2
            rows = slice(t * 128, (t + 1) * 128)
            P.kp = None
            P.dma("sp", lambda q: q.dma_start(out=qk[b][:], in_=D["dn_qkv"][rows, :]), r=["dn_qkv"], w=[("qk", b)])
            P.dma("sp", lambda q: q.dma_start(out=scs[b][:], in_=D["dn_sc"][rows, :]), r=["dn_sc"], w=[("sc", b)])
            gens = [(h, unit(h, t, b)) for h in range(2)]
            while gens:
                for item in list(gens):
                    P.kp = item[0]
                    try:
                        next(item[1])
                    except StopIteration:
                        gens.remove(item)
            P.kp = None
        P.kp = None
import os as _os2
SWCUT = int(_os2.environ.get('SWCUT', '99'))
SWOPS = int(_os2.environ.get('SWOPS', '99'))
SWSKIP0 = int(_os2.environ.get('SWSKIP0', '0'))
SWALT = int(_os2.environ.get('SWALT', '0'))
SWNOCP = int(_os2.environ.get('SWNOCP', '0'))
SWMETA = int(_os2.environ.get('SWMETA', '1'))
def swa_pass(P, nc, D, C, nt=NT):
    NB = nt - 1
    scale = float(128 ** -0.5)
    with ExitStack() as ES:
        kt = ES.enter_context(nc.sbuf_tensor(_u("kt"), [128, 4, 512], BF16))
        KT = ES.enter_context(nc.sbuf_tensor(_u("KT"), [128, 4, 128], BF16))
        mq = ES.enter_context(nc.sbuf_tensor(_u("mq"), [16, 512], BF16))
        KTm = ES.enter_context(nc.sbuf_tensor(_u("KTm"), [128, 16], BF16))
        bias = ES.enter_context(nc.sbuf_tensor(_u("bias"), [128, 3, 2, 400], F32))
        mbias = ES.enter_context(nc.sbuf_tensor(_u("mbias"), [16, 144], F32))
        sink = ES.enter_context(nc.sbuf_tensor(_u("sink"), [128, 2], F32))
        Ss = ES.enter_context(nc.sbuf_tensor(_u("Ss"), [128, 400], F32))
        Pb = ES.enter_context(nc.sbuf_tensor(_u("Pb"), [128, 400], BF16))
        PT = ES.enter_context(nc.sbuf_tensor(_u("PT"), [128, 4, 128], BF16))
        QT = ES.enter_context(nc.sbuf_tensor(_u("QT"), [128, 128], BF16))
        ob = ES.enter_context(nc.sbuf_tensor(_u("ob"), [128, 128], BF16))
        ss = ES.enter_context(nc.sbuf_tensor(_u("ss"), [128, 16], F32))
        zt = ES.enter_context(nc.sbuf_tensor(_u("zt"), [128, 256], BF16))
        pS = ES.enter_context(nc.psum_tensor(_u("pS"), [128, 512], F32))
        pP = ES.enter_context(nc.psum_tensor(_u("pP"), [128, 512], BF16))
        pO = ES.enter_context(nc.psum_tensor(_u("pO"), [128, 512], F32))
        pK = ES.enter_context(nc.psum_tensor(_u("pK"), [128, 512], BF16))
        identb = C["identb"]
        P.dma("sp", lambda q: q.dma_start(out=bias[:], in_=D["swbias"]), w=["bias"])
        P.dma("sp", lambda q: q.dma_start(out=mbias[:], in_=D["swmbias"]), w=["mbias"])
        P.dma("sp", lambda q: q.dma_start(out=sink[:], in_=D["sinkbc"]), w=["sink"])
        P.dma("sp", lambda q: q.dma_start(out=mq[:], in_=D["sw_qkv"][112:128, :]), r=["sw_qkv"], w=["mq"])
        P.op("pool", lambda e: e.memset(zt[:], 0.0), w=["zt"])
        P.dma("sp", lambda q: q.dma_start(out=D["oab"][0:112, 256:512], in_=zt[0:112, :]), r=["zt"], w=["oab"])
        P.op("pe", lambda e: e.transpose(pK[:, 0:16], mq[:, 256:384], identb[0:16, 0:16]), r=["mq", "identb"], w=["pK"])
        P.op("act", lambda e: e.copy(out=KTm[:], in_=pK[:, 0:16]), r=["pK"], w=["KTm"])

        def load(t):
            s = t % 4
            P.dma("sp", lambda q: q.dma_start(out=kt[:, s, :], in_=D["sw_qkv"][t * 128:(t + 1) * 128, :]), r=["sw_qkv"], w=[("kt", s)])
            P.op("pe", lambda e: e.transpose(pK[:, 0:128], kt[:, s, 256:384], identb[:]), r=[("kt", s), "identb"], w=["pK"])
            P.op("act", lambda e: e.copy(out=KT[:, s, :], in_=pK[:, 0:128]), r=["pK"], w=[("KT", s)])

        def attend(np_, qsrc, qkey, keysets, bias_ap, sink_ap, out_ap):
            ntot = sum(k[2] for k in keysets)
            deps = [qkey] + [dd for k in keysets for dd in k[3]]
            P.op("pe", lambda e: e.transpose(pP[:, 0:np_], qsrc, identb[0:np_, 0:np_]), r=[qkey, "identb"], w=["pP"])
            P.op("act", lambda e: e.copy(out=QT[:, 0:np_], in_=pP[:, 0:np_]), r=["pP"], w=["QT"])
            if SWCUT < 2:
                return
            fns = []
            c0 = 0
            for (ka, va, nk, _) in keysets:
                fns.append(mm(pS[0:np_, c0:c0 + nk], QT[:, 0:np_], ka))
                c0 += nk
            P.group("pe", fns, r=["QT"] + deps, w=["pS"])
            if SWCUT < 3:
                return
            if SWOPS < 1:
                return
            P.op("dve", lambda e: e.scalar_tensor_tensor(out=Ss[0:np_, 0:ntot], in0=pS[0:np_, 0:ntot], scalar=scale, in1=bias_ap, op0=ALU.mult, op1=ALU.add),
                 r=["pS", "bias", "mbias"], w=["Ss"])
            if SWOPS < 2:
                return
            P.op("dve", lambda e: e.reduce_max(out=ss[0:np_, 0:1], in_=Ss[0:np_, 0:ntot], axis=AX.X), r=["Ss"], w=["mx"])
            if SWOPS < 3:
                return
            P.op("dve", lambda e: e.tensor_tensor(out=ss[0:np_, 1:2], in0=ss[0:np_, 0:1], in1=sink_ap, op=ALU.max), r=["mx", "sink"], w=["m"])
            if SWOPS < 4:
                return
            P.op("dve", lambda e: e.tensor_scalar(out=ss[0:np_, 2:3], in0=ss[0:np_, 1:2], scalar1=-1.0, scalar2=None, op0=ALU.mult), r=["m"], w=["negm"])
            if SWOPS < 5:
                return
            P.op("act", lambda e: e.activation(out=Pb[0:np_, 0:ntot], in_=Ss[0:np_, 0:ntot], func=AF.Exp, bias=ss[0:np_, 2:3], accum_out=ss[0:np_, 3:4]),
                 r=["Ss", "negm"], w=["Pb", "rs"])
            if SWOPS < 6:
                return
            P.op("act", lambda e: e.activation(out=ss[0:np_, 4:5], in_=ss[0:np_, 2:3], func=AF.Exp, bias=sink_ap), r=["negm", "sink"], w=["es"])
            if SWOPS < 7:
                return
            P.op("dve", lambda e: e.tensor_tensor(out=ss[0:np_, 5:6], in0=ss[0:np_, 3:4], in1=ss[0:np_, 4:5], op=ALU.add), r=["rs", "es"], w=["den"])
            if SWOPS < 8:
                return
            P.op("dve", lambda e: e.reciprocal(out=ss[0:np_, 6:7], in_=ss[0:np_, 5:6]), r=["den"], w=["rden"])
            if SWCUT < 4:
                return
            fns = []
            c0 = 0
            for j, (ka, va, nk, _) in enumerate(keysets):
                if not (SWSKIP0 and nk == 16):
                    fns.append(lambda e, j=j, c0=c0, nk=nk: e.transpose(pP[0:nk, j * 128:j * 128 + np_], (kt[0:np_, 0, c0:c0 + nk] if SWALT else Pb[0:np_, c0:c0 + nk]), identb[0:np_, 0:np_]))
                c0 += nk
            P.group("pe", fns, r=["Pb", "identb", "QT"], w=["pP"])
            for j, (ka, va, nk, _) in enumerate(keysets):
                if SWNOCP or (SWSKIP0 and nk == 16):
                    continue
                P.op("act", (lambda e, j=j, nk=nk: e.copy(out=PT[0:nk, j, 0:np_], in_=pP[0:nk, j * 128:j * 128 + np_])), r=["pP"], w=["PT"])
            if SWCUT < 5:
                return
            fns = []
            for j, (ka, va, nk, _) in enumerate(keysets):
                fns.append(mm(pO[0:np_, 0:128], PT[0:nk, j, 0:np_], va, start=(j == 0), stop=(j == len(keysets) - 1)))
            P.group("pe", fns, r=["PT"] + deps, w=["pO"])
            P.op("act", lambda e: e.activation(out=ob[0:np_, :], in_=pO[0:np_, 0:128], func=AF.Copy, scale=ss[0:np_, 6:7]), r=["pO", "rden"], w=["ob"])
            P.dma("sp", lambda q: q.dma_start(out=out_ap, in_=ob[0:np_, :]), r=["ob"], w=["oab"])

        load(0)
        load(1)
        meta_ks = (KTm[:, 0:16], mq[0:16, 384:512], 16, ["KTm", "mq"])
        for h in (range(2) if SWMETA else []):
            attend(16, mq[0:16, h * 128:(h + 1) * 128], "mq",
                   [(KT[:, 1, :], kt[:, 1, 384:512], 128, [("KT", 1), ("kt", 1)]), meta_ks],
                   mbias[:, :], sink[0:16, h:h + 1], D["oab"][112:128, 256 + h * 128:256 + (h + 1) * 128])
        for n in range(NB):
            t = n + 1
            if t + 1 < nt:
                load(t + 1)
            var = 0 if n == 0 else (2 if n == NB - 1 else 1)
            tn = t + 1 if t + 1 < nt else t
            sl = [(t - 1) % 4, t % 4, tn % 4]
            for h in range(2):
                ks = [(KT[:, s, :], kt[:, s, 384:512], 128, [("KT", s), ("kt", s)]) for s in sl] + [meta_ks]
                attend(128, kt[:, t % 4, h * 128:(h + 1) * 128], ("kt", t % 4), ks, bias[:, var, h, :], sink[:, h:h + 1],
                       D["oab"][t * 128:(t + 1) * 128, 256 + h * 128:256 + (h + 1) * 128])
def allgather(P, src, dst, rkeys, wkeys):
    P.dma("pool", lambda q: q.collective_compute("AllGather", ALU.bypass, replica_groups=[list(range(8))],
                                                 ins=[src.opt()], outs=[dst.opt()]), r=rkeys, w=wkeys, inc=1)


def transpose_into(P, src_fn, dstT, nch, tp, identb, rkeys, wkey, evac=("act", "dve")):
    for g in range((nch + 3) // 4):
        tb = g % 2
        n = min(4, nch - 4 * g)
        P.group("pe", [(lambda e, j=j: e.transpose(tp[tb][:, j * 128:(j + 1) * 128], src_fn(4 * g + j), identb[:])) for j in range(n)],
                r=rkeys + ["identb"], w=[("tp", tb)])
        src = tp[tb][:, 0:n * 128].rearrange("p (a b) -> p a b", b=128)
        if evac[g % 2] == "act":
            P.op("act", lambda e: e.copy(out=dstT[:, 4 * g:4 * g + n, :], in_=src), r=[("tp", tb)], w=[wkey])
        else:
            P.op("dve", lambda e: e.tensor_copy(out=dstT[:, 4 * g:4 * g + n, :], in_=src), r=[("tp", tb)], w=[wkey])


def phase_B1(P, nc, D, C, nt=NT):
    with ExitStack() as ES:
        Wga = ES.enter_context(nc.sbuf_tensor(_u("Wga"), [128, 32, 512], BF16))
        Wgb = ES.enter_context(nc.sbuf_tensor(_u("Wgb"), [128, 32, 512], BF16))
        Wa = ES.enter_context(nc.sbuf_tensor(_u("Wa"), [128, 16, 512], BF16))
        Wb = ES.enter_context(nc.sbuf_tensor(_u("Wb"), [128, 16, 512], BF16))
        xt0 = ES.enter_context(nc.sbuf_tensor(_u("xt0"), [128, 4096], F32))
        xt1 = ES.enter_context(nc.sbuf_tensor(_u("xt1"), [128, 4096], F32))
        xn = ES.enter_context(nc.sbuf_tensor(_u("xn"), [128, 4096], BF16))
        xT = ES.enter_context(nc.sbuf_tensor(_u("xT"), [128, 32, 128], BF16))
        ob0 = ES.enter_context(nc.sbuf_tensor(_u("ob0"), [128, 8, 512], BF16))
        ob1 = ES.enter_context(nc.sbuf_tensor(_u("ob1"), [128, 8, 512], BF16))
        oT = ES.enter_context(nc.sbuf_tensor(_u("oT"), [128, 32, 128], BF16))
        sg = ES.enter_context(nc.sbuf_tensor(_u("sg"), [128, 2, 512], F32))
        mxb = ES.enter_context(nc.sbuf_tensor(_u("mx"), [128, 512], BF16))
        sm = ES.enter_context(nc.sbuf_tensor(_u("sm"), [128, 8], F32))
        w1c = ES.enter_context(nc.sbuf_tensor(_u("w1c"), [128, 32], F32))
        tp0 = ES.enter_context(nc.psum_tensor(_u("tp0"), [128, 512], BF16))
        tp1 = ES.enter_context(nc.psum_tensor(_u("tp1"), [128, 512], BF16))
        p0 = ES.enter_context(nc.psum_tensor(_u("p0"), [128, 512], F32))
        p1 = ES.enter_context(nc.psum_tensor(_u("p1"), [128, 512], F32))
        p2 = ES.enter_context(nc.psum_tensor(_u("p2"), [128, 512], F32))
        p3 = ES.enter_context(nc.psum_tensor(_u("p3"), [128, 512], F32))
        xt = [xt0, xt1]
        ob = [ob0, ob1]
        tp = [tp0, tp1]
        identb = C["identb"]
        P.dma("sp", lambda q: q.dma_start(out=w1c[:], in_=D["w1c"]), w=["w1c"])
        for wi, (Wt, nm) in enumerate([(Wga, "wga"), (Wgb, "wgb")]):
            for kc in range(32):
                b = kc % 2
                P.dma("sp", lambda q: q.dma_start(out=xt[b][:, 0:512], in_=D[nm][kc * 128:(kc + 1) * 128, :]), w=[("xt", b)])
                P.op("act", lambda e: e.activation(out=Wt[:, kc, :], in_=xt[b][:, 0:512], func=AF.Copy, scale=w1c[:, kc:kc + 1]),
                     r=[("xt", b), "w1c"], w=["W" + nm])
        P.dma("pool", lambda q: q.dma_start(out=Wa[:], in_=D["wa"].rearrange("(kc p) n -> p kc n", p=128)), w=["Wwa"])
        P.dma("pool", lambda q: q.dma_start(out=Wb[:], in_=D["wb"].rearrange("(kc p) n -> p kc n", p=128)), w=["Wwb"])
        for t in range(nt):
            b = t % 2
            rows = slice(t * 128, (t + 1) * 128)
            P.dma("sp", lambda q: q.dma_start(out=xt[b][:], in_=D["xp"][rows, :]), w=[("xt", b)])
            for r in range(8):
                P.dma("sp", lambda q: q.dma_start(out=ob[b][:, r, :], in_=D["oab_all"][r * NR + t * 128:r * NR + (t + 1) * 128, :]), r=["oab_all"], w=[("ob", b)])
            P.op("act", lambda e: e.activation(out=xn[:], in_=xt[b][:], func=AF.Square, accum_out=sm[:, 0:1]), r=[("xt", b)], w=["xn", "ss"])
            P.op("act", lambda e: e.activation(out=sm[:, 1:2], in_=sm[:, 0:1], func=AF.Sqrt, bias=EPS, scale=1.0 / 4096), r=["ss"], w=["rt"])
            P.op("dve", lambda e: e.reciprocal(out=sm[:, 2:3], in_=sm[:, 1:2]), r=["rt"], w=["rstd"])
            P.op("act", lambda e: e.activation(out=xn[:], in_=xt[b][:], func=AF.Copy, scale=sm[:, 2:3]), r=[("xt", b), "rstd"], w=["xn"])
            transpose_into(P, lambda i: xn[:, i * 128:(i + 1) * 128], xT, 32, tp, identb, ["xn"], "xT")
            transpose_into(P, lambda i: ob[b][:, (i % 16) // 2, (i // 16) * 256 + (i % 2) * 128:(i // 16) * 256 + (i % 2) * 128 + 128], oT, 32, tp, identb, [("ob", b)], "oT")
            P.group("pe", [mm(p0[:], xT[:, kc, :], Wga[:, kc, :], kc == 0, kc == 31) for kc in range(32)], r=["xT", "Wwga"], w=["p0"])
            P.group("pe", [mm(p1[:], xT[:, kc, :], Wgb[:, kc, :], kc == 0, kc == 31) for kc in range(32)], r=["xT", "Wwgb"], w=["p1"])
            P.group("pe", [mm(p2[:], oT[:, kc, :], Wa[:, kc, :], kc == 0, kc == 15) for kc in range(16)], r=["oT", "Wwa"], w=["p2"])
            P.group("pe", [mm(p3[:], oT[:, 16 + kc, :], Wb[:, kc, :], kc == 0, kc == 15) for kc in range(16)], r=["oT", "Wwb"], w=["p3"])
            P.op("act", lambda e: e.activation(out=sg[:, 0, :], in_=p0[:], func=AF.Sigmoid), r=["p0"], w=[("sg", 0)])
            P.op("act", lambda e: e.activation(out=sg[:, 1, :], in_=p1[:], func=AF.Sigmoid), r=["p1"], w=[("sg", 1)])
            P.op("dve", lambda e: e.tensor_tensor(out=sg[:, 0, :], in0=sg[:, 0, :], in1=p2[:], op=ALU.mult), r=[("sg", 0), "p2"], w=[("sg", 0)])
            P.op("dve", lambda e: e.tensor_tensor(out=sg[:, 1, :], in0=sg[:, 1, :], in1=p3[:], op=ALU.mult), r=[("sg", 1), "p3"], w=[("sg", 1)])
            P.op("pool", lambda e: e.tensor_tensor(out=mxb[:], in0=sg[:, 0, :], in1=sg[:, 1, :], op=ALU.add), r=[("sg", 0), ("sg", 1)], w=["mxb"])
            P.dma("sp", lambda q: q.dma_start(out=D["mixed"][rows, :], in_=mxb[:]), r=["mxb"], w=["mixed"])


def phase_B2(P, nc, D, C, nt=NT):
    with ExitStack() as ES:
        Wo = ES.enter_context(nc.sbuf_tensor(_u("Wo"), [128, 32, 512], BF16))
        mb0 = ES.enter_context(nc.sbuf_tensor(_u("mb0"), [128, 8, 512], BF16))
        mb1 = ES.enter_context(nc.sbuf_tensor(_u("mb1"), [128, 8, 512], BF16))
        mT = ES.enter_context(nc.sbuf_tensor(_u("mT"), [128, 32, 128], BF16))
        xc0 = ES.enter_context(nc.sbuf_tensor(_u("xc0"), [128, 512], F32))
        xc1 = ES.enter_context(nc.sbuf_tensor(_u("xc1"), [128, 512], F32))
        h2o = ES.enter_context(nc.sbuf_tensor(_u("h2o"), [128, 512], F32))
        tp0 = ES.enter_context(nc.psum_tensor(_u("tp0"), [128, 512], BF16))
        tp1 = ES.enter_context(nc.psum_tensor(_u("tp1"), [128, 512], BF16))
        p0 = ES.enter_context(nc.psum_tensor(_u("p0"), [128, 512], F32))
        mb = [mb0, mb1]
        xc = [xc0, xc1]
        tp = [tp0, tp1]
        identb = C["identb"]
        P.dma("pool", lambda q: q.dma_start(out=Wo[:], in_=D["wo"].rearrange("(kc p) n -> p kc n", p=128)), w=["Wo"])
        for t in range(nt):
            b = t % 2
            rows = slice(t * 128, (t + 1) * 128)
            for r in range(8):
                P.dma("sp", lambda q: q.dma_start(out=mb[b][:, r, :], in_=D["mixed_all"][r * NR + t * 128:r * NR + (t + 1) * 128, :]), r=["mixed_all"], w=[("mb", b)])
            P.dma("sp", lambda q: q.dma_start(out=xc[b][:], in_=D["xcol"][rows, :]), w=[("xc", b)])
            transpose_into(P, lambda i: mb[b][:, i // 4, (i % 4) * 128:(i % 4) * 128 + 128], mT, 32, tp, identb, [("mb", b)], "mT")
            P.group("pe", [mm(p0[:], mT[:, kc, :], Wo[:, kc, :], kc == 0, kc == 31) for kc in range(32)], r=["mT", "Wo"], w=["p0"])
            P.op("dve", lambda e: e.tensor_tensor(out=h2o[:], in0=xc[b][:], in1=p0[:], op=ALU.add), r=[("xc", b), "p0"], w=["h2o"])
            P.dma("sp", lambda q: q.dma_start(out=D["h2c"][rows, :], in_=h2o[:]), r=["h2o"], w=["h2c"])


def phase_B3(P, nc, D, C, affs, nt=NT):
    with ExitStack() as ES:
        h0 = ES.enter_context(nc.sbuf_tensor(_u("h0"), [128, 4096], F32))
        h1 = ES.enter_context(nc.sbuf_tensor(_u("h1"), [128, 4096], F32))
        hnf = ES.enter_context(nc.sbuf_tensor(_u("hnf"), [128, 4096], F32))
        w2 = ES.enter_context(nc.sbuf_tensor(_u("w2"), [128, 4096], F32))
        hnb = ES.enter_context(nc.sbuf_tensor(_u("hnb"), [128, 4096], BF16))
        hT = ES.enter_context(nc.sbuf_tensor(_u("hT"), [128, 32, 128], F32))
        wr = ES.enter_context(nc.sbuf_tensor(_u("wr"), [128, 32, 16], F32))
        sm = ES.enter_context(nc.sbuf_tensor(_u("sm"), [128, 32], F32))
        tq0 = ES.enter_context(nc.psum_tensor(_u("tq0"), [128, 512], F32))
        tq1 = ES.enter_context(nc.psum_tensor(_u("tq1"), [128, 512], F32))
        pl = ES.enter_context(nc.psum_tensor(_u("pl"), [128, 512], F32))
        hh = [h0, h1]
        tq = [tq0, tq1]
        identf = C["identf"]
        P.dma("sp", lambda q: q.dma_start(out=w2[:], in_=D["w2bc"]), w=["w2"])
        P.dma("sp", lambda q: q.dma_start(out=wr[:], in_=D["wr"].rearrange("(kc p) e -> p kc e", p=128)), w=["wr"])
        for t in range(nt):
            b = t % 2
            rows = slice(t * 128, (t + 1) * 128)
            for r in range(8):
                P.dma("sp", lambda q: q.dma_start(out=hh[b][:, r * 512:(r + 1) * 512], in_=D["h2_all"][r * NR + t * 128:r * NR + (t + 1) * 128, :]), r=["h2_all"], w=[("hh", b)])
            P.op("act", lambda e: e.activation(out=hnb[:], in_=hh[b][:], func=AF.Square, accum_out=sm[:, 0:1]), r=[("hh", b)], w=["hnb", "ss"])
            P.op("act", lambda e: e.activation(out=sm[:, 1:2], in_=sm[:, 0:1], func=AF.Sqrt, bias=EPS, scale=1.0 / 4096), r=["ss"], w=["rt"])
            P.op("dve", lambda e: e.reciprocal(out=sm[:, 2:3], in_=sm[:, 1:2]), r=["rt"], w=["rstd"])
            P.op("dve", lambda e: e.scalar_tensor_tensor(out=hnf[:], in0=hh[b][:], scalar=sm[:, 2:3], in1=w2[:], op0=ALU.mult, op1=ALU.mult), r=[("hh", b), "rstd", "w2"], w=["hnf"])
            P.op("act", lambda e: e.copy(out=hnb[:], in_=hnf[:]), r=["hnf"], w=["hnb"])
            P.dma("sp", lambda q: q.dma_start(out=D["hn2"][rows, :], in_=hnb[:]), r=["hnb"], w=["hn2"])
            for g in range(8):
                tb = g % 2
                P.group("pe", [(lambda e, j=j: e.transpose(tq[tb][:, j * 128:(j + 1) * 128], hnf[:, (4 * g + j) * 128:(4 * g + j + 1) * 128], identf[:])) for j in range(4)],
                        r=["hnf", "identf"], w=[("tq", tb)])
                src = tq[tb][:].rearrange("p (a b) -> p a b", b=128)
                if g % 2 == 0:
                    P.op("act", lambda e: e.copy(out=hT[:, 4 * g:4 * g + 4, :], in_=src), r=[("tq", tb)], w=["hT"])
                else:
                    P.op("dve", lambda e: e.tensor_copy(out=hT[:, 4 * g:4 * g + 4, :], in_=src), r=[("tq", tb)], w=["hT"])
            P.group("pe", [mm(pl[:, 0:16], hT[:, kc, :], wr[:, kc, :], kc == 0, kc == 31) for kc in range(32)], r=["hT", "wr"], w=["pl"])
            P.op("dve", lambda e: e.reduce_max(out=sm[:, 3:4], in_=pl[:, 0:16], axis=AX.X), r=["pl"], w=["mx"])
            P.op("dve", lambda e: e.tensor_scalar(out=sm[:, 4:5], in0=sm[:, 3:4], scalar1=-1.0, scalar2=None, op0=ALU.mult), r=["mx"], w=["negm"])
            P.op("act", lambda e: e.activation(out=sm[:, 8:24], in_=pl[:, 0:16], func=AF.Exp, bias=sm[:, 4:5], accum_out=sm[:, 5:6]), r=["pl", "negm"], w=["ex", "es"])
            P.op("dve", lambda e: e.reciprocal(out=sm[:, 6:7], in_=sm[:, 5:6]), r=["es"], w=["rs"])
            P.op("dve", lambda e: e.tensor_scalar(out=affs[:, t, :], in0=sm[:, 8:24], scalar1=sm[:, 6:7], scalar2=None, op0=ALU.mult), r=["ex", "rs"], w=["affs"])


CAP = 1026
BIG = float(1 << 20)


def phase_topk(P, nc, D, C, affs, sloti, nt=NT):
    n3 = nt * 16
    with ExitStack() as ES:
        cmp_ = ES.enter_context(nc.sbuf_tensor(_u("cmp"), [128, nt, 16], F32))
        val = ES.enter_context(nc.sbuf_tensor(_u("val"), [128, nt, 16], F32))
        pos = ES.enter_context(nc.sbuf_tensor(_u("pos"), [128, nt, 16], F32))
        tt = ES.enter_context(nc.sbuf_tensor(_u("tt"), [128, nt, 16], F32))
        cs = ES.enter_context(nc.sbuf_tensor(_u("cs"), [128, nt, 16], F32))
        one = ES.enter_context(nc.sbuf_tensor(_u("one"), [128, nt], F32))
        lst = ES.enter_context(nc.sbuf_tensor(_u("lst"), [128, 128], F32))
        bb = ES.enter_context(nc.sbuf_tensor(_u("b"), [128, 8, 16], F32))
        pc = ES.enter_context(nc.psum_tensor(_u("pc"), [128, 512], F32))
        pw0 = ES.enter_context(nc.psum_tensor(_u("pw0"), [128, 512], F32))
        pw1 = ES.enter_context(nc.psum_tensor(_u("pw1"), [128, 512], F32))
        pw2 = ES.enter_context(nc.psum_tensor(_u("pw2"), [128, 512], F32))
        onesf = C["onesf"]
        P.dma("sp", lambda q: q.dma_start(out=val[:], in_=D["validbc"]), w=["val"])
        P.dma("sp", lambda q: q.dma_start(out=lst[:], in_=D["lstrict"]), w=["lst"])
        P.op("dve", lambda e: e.tensor_tensor(out=affs[:], in0=affs[:], in1=val[:], op=ALU.mult), r=["affs", "val"], w=["affs"])
        P.op("pool", lambda e: e.memset(bb[:, 0, :], 0.0), w=["lo"])
        P.op("pool", lambda e: e.memset(bb[:, 1, :], 1.0), w=["hi"])
        P.op("pool", lambda e: e.memset(one[:], 1.0), w=["one"])
        affs_et = affs[:].rearrange("p t e -> p e t")

        def bc(ap):
            return ap.unsqueeze(1).broadcast_to([128, nt, 16])

        for it in range(34):
            P.op("dve", lambda e: e.tensor_tensor(out=bb[:, 2, :], in0=bb[:, 0, :], in1=bb[:, 1, :], op=ALU.add), r=["lo", "hi"], w=["mid"])
            P.op("dve", lambda e: e.tensor_scalar(out=bb[:, 2, :], in0=bb[:, 2, :], scalar1=0.5, scalar2=None, op0=ALU.mult), r=["mid"], w=["mid"])
            P.op("dve", lambda e: e.tensor_tensor(out=cmp_[:], in0=affs[:], in1=bc(bb[:, 2, :]), op=ALU.is_ge), r=["affs", "mid"], w=["cmp"])
            P.op("dve", lambda e: e.tensor_reduce(out=bb[:, 3, :], in_=cmp_[:].rearrange("p t e -> p e t"), axis=AX.X, op=ALU.add), r=["cmp"], w=["cnt"])
            P.op("pe", mm(pc[:, 0:16], onesf[:], bb[:, 3, :]), r=["cnt", "onesf"], w=["pc"])
            P.op("dve", lambda e: e.tensor_single_scalar(out=bb[:, 4, :], in_=pc[:, 0:16], scalar=CAP - 0.5, op=ALU.is_ge), r=["pc"], w=["ge"])
            P.op("dve", lambda e: e.tensor_tensor(out=bb[:, 5, :], in0=bb[:, 2, :], in1=bb[:, 0, :], op=ALU.subtract), r=["mid", "lo"], w=["d1"])
            P.op("dve", lambda e: e.tensor_tensor(out=bb[:, 5, :], in0=bb[:, 5, :], in1=bb[:, 4, :], op=ALU.mult), r=["d1", "ge"], w=["d1"])
            P.op("dve", lambda e: e.tensor_tensor(out=bb[:, 6, :], in0=bb[:, 1, :], in1=bb[:, 2, :], op=ALU.subtract), r=["mid", "hi"], w=["d2"])
            P.op("dve", lambda e: e.tensor_tensor(out=bb[:, 6, :], in0=bb[:, 6, :], in1=bb[:, 4, :], op=ALU.mult), r=["d2", "ge"], w=["d2"])
            P.op("dve", lambda e: e.tensor_tensor(out=bb[:, 0, :], in0=bb[:, 0, :], in1=bb[:, 5, :], op=ALU.add), r=["lo", "d1"], w=["lo"])
            P.op("dve", lambda e: e.tensor_tensor(out=bb[:, 1, :], in0=bb[:, 2, :], in1=bb[:, 6, :], op=ALU.add), r=["mid", "d2"], w=["hi"])
        P.op("dve", lambda e: e.tensor_tensor(out=cmp_[:], in0=affs[:], in1=bc(bb[:, 0, :]), op=ALU.is_ge), r=["affs", "lo"], w=["cmp"])
        cf = cmp_[:].rearrange("p t e -> p (t e)")
        pws = [pw0, pw1, pw2]
        for i in range(3):
            c0, c1 = i * 512, min(n3, (i + 1) * 512)
            P.op("pe", mm(pws[i][:, 0:c1 - c0], lst[:], cf[:, c0:c1]), r=["cmp", "lst"], w=[("pw", i)])
            P.op("act", lambda e: e.copy(out=pos[:].rearrange("p t e -> p (t e)")[:, c0:c1], in_=pws[i][:, 0:c1 - c0]), r=[("pw", i)], w=["pos"])
        for i in range(3):
            c0, c1 = i * 512, min(n3, (i + 1) * 512)
            P.op("pe", mm(pws[i][:, 0:c1 - c0], onesf[:], cf[:, c0:c1]), r=["cmp", "onesf", "pos"], w=[("pw", i)])
            P.op("act", lambda e: e.copy(out=tt[:].rearrange("p t e -> p (t e)")[:, c0:c1], in_=pws[i][:, 0:c1 - c0]), r=[("pw", i)], w=["tt"])
        for ex in range(16):
            P.op("dve", lambda e: e.tensor_tensor_scan(out=cs[:, :, ex], data0=one[:], data1=tt[:, :, ex], initial=0.0, op0=ALU.mult, op1=ALU.add), r=["tt", "one"], w=["cs"])
        P.op("dve", lambda e: e.tensor_tensor(out=cs[:], in0=cs[:], in1=tt[:], op=ALU.subtract), r=["cs", "tt"], w=["cs"])
        P.op("dve", lambda e: e.tensor_tensor(out=pos[:], in0=pos[:], in1=cs[:], op=ALU.add), r=["pos", "cs"], w=["pos"])
        P.op("dve", lambda e: e.tensor_scalar(out=cmp_[:], in0=cmp_[:], scalar1=-BIG, scalar2=BIG, op0=ALU.mult, op1=ALU.add), r=["cmp"], w=["cmp"])
        P.op("dve", lambda e: e.tensor_tensor(out=pos[:], in0=pos[:], in1=cmp_[:], op=ALU.add), r=["pos", "cmp"], w=["pos"])
        P.op("dve", lambda e: e.tensor_scalar(out=cmp_[:], in0=pos[:], scalar1=CAP - 0.5, scalar2=BIG, op0=ALU.is_ge, op1=ALU.mult), r=["pos", "cmp"], w=["cmp"])
        P.op("dve", lambda e: e.tensor_tensor(out=pos[:], in0=pos[:], in1=cmp_[:], op=ALU.add), r=["pos", "cmp"], w=["pos"])
        P.op("dve", lambda e: e.tensor_copy(out=sloti[:], in_=pos[:]), r=["pos"], w=["sloti"])
        P.dma("sp", lambda q: q.dma_start(out=D["slot_tab"].rearrange("(t p) e -> p t e", p=128), in_=pos[:]), r=["pos"], w=["slot_tab"])
        P.dma("sp", lambda q: q.dma_start(out=D["aff_tab"].rearrange("(t p) e -> p t e", p=128), in_=affs[:]), r=["affs"], w=["aff_tab"])
_BR = {}
def BREG(nc, v):
    k = (id(nc), v)
    if k not in _BR:
        _BR[k] = nc.gpsimd.to_reg(v)
    return _BR[k]

NS = 1152


def phase_C(P, nc, D, C, sloti, nt=NT):
    identb = C["identb"]
    with ExitStack() as ES:
        hb0 = ES.enter_context(nc.sbuf_tensor(_u("hb0"), [128, 4096], BF16))
        hb1 = ES.enter_context(nc.sbuf_tensor(_u("hb1"), [128, 4096], BF16))
        hb = [hb0, hb1]
        for t in range(nt):
            b = t % 2
            P.dma("sp", lambda q: q.dma_start(out=hb[b][:], in_=D["hn2"][t * 128:(t + 1) * 128, :]), r=["hn2"], w=[("hb", b)])
            for j in range(2):
                P.dma("pool", lambda q: q.indirect_dma_start(out=D["Xe%d" % j], out_offset=bass.IndirectOffsetOnAxis(ap=sloti[:, t, j:j + 1], axis=0),
                                                             in_=hb[b][:], in_offset=None, bounds_check=BREG(nc, CAP - 1), oob_is_err=False),
                      r=[("hb", b), "sloti"], w=["Xe%d" % j])
    P.barrier()
    with ExitStack() as ES:
        XeT = ES.enter_context(nc.sbuf_tensor(_u("XeT"), [128, 32, NS], BF16))
        hT = ES.enter_context(nc.sbuf_tensor(_u("hT"), [128, 16, NS], BF16))
        xl0 = ES.enter_context(nc.sbuf_tensor(_u("xl0"), [128, 4096], BF16))
        xl1 = ES.enter_context(nc.sbuf_tensor(_u("xl1"), [128, 4096], BF16))
        wg0 = ES.enter_context(nc.sbuf_tensor(_u("wg0"), [128, 32, 128], BF16))
        wg1 = ES.enter_context(nc.sbuf_tensor(_u("wg1"), [128, 32, 128], BF16))
        wu0 = ES.enter_context(nc.sbuf_tensor(_u("wu0"), [128, 32, 128], BF16))
        wu1 = ES.enter_context(nc.sbuf_tensor(_u("wu1"), [128, 32, 128], BF16))
        wd0 = ES.enter_context(nc.sbuf_tensor(_u("wd0"), [128, 16, 512], BF16))
        wd1 = ES.enter_context(nc.sbuf_tensor(_u("wd1"), [128, 16, 512], BF16))
        sil = ES.enter_context(nc.sbuf_tensor(_u("sil"), [128, 384], F32))
        yo0 = ES.enter_context(nc.sbuf_tensor(_u("yo0"), [128, 512], BF16))
        yo1 = ES.enter_context(nc.sbuf_tensor(_u("yo1"), [128, 512], BF16))
        tp0 = ES.enter_context(nc.psum_tensor(_u("tp0"), [128, 512], BF16))
        tp1 = ES.enter_context(nc.psum_tensor(_u("tp1"), [128, 512], BF16))
        pg0 = ES.enter_context(nc.psum_tensor(_u("pg0"), [128, 512], F32))
        pg1 = ES.enter_context(nc.psum_tensor(_u("pg1"), [128, 512], F32))
        pu0 = ES.enter_context(nc.psum_tensor(_u("pu0"), [128, 512], F32))
        pu1 = ES.enter_context(nc.psum_tensor(_u("pu1"), [128, 512], F32))
        py0 = ES.enter_context(nc.psum_tensor(_u("py0"), [128, 512], F32))
        py1 = ES.enter_context(nc.psum_tensor(_u("py1"), [128, 512], F32))
        xl = [xl0, xl1]
        tp = [tp0, tp1]
        wgs, wus, wds = [wg0, wg1], [wu0, wu1], [wd0, wd1]
        pgs, pus, pys, yos = [pg0, pg1], [pu0, pu1], [py0, py1], [yo0, yo1]
        cnt = 0
        for j in range(2):
            for st in range(9):
                b = st % 2
                P.dma("sp", lambda q: q.dma_start(out=xl[b][:], in_=D["Xe%d" % j][st * 128:(st + 1) * 128, :]), r=["Xe%d" % j], w=[("xl", b)])
                for g in range(8):
                    tb = g % 2
                    P.group("pe", [(lambda e, jj=jj: e.transpose(tp[tb][:, jj * 128:(jj + 1) * 128], xl[b][:, (4 * g + jj) * 128:(4 * g + jj + 1) * 128], identb[:])) for jj in range(4)],
                            r=[("xl", b), "identb"], w=[("tp", tb)])
                    src = tp[tb][:].rearrange("p (a b) -> p a b", b=128)
                    if g % 2 == 0:
                        P.op("act", lambda e: e.copy(out=XeT[:, 4 * g:4 * g + 4, st * 128:(st + 1) * 128], in_=src), r=[("tp", tb)], w=["XeT"])
                    else:
                        P.op("dve", lambda e: e.tensor_copy(out=XeT[:, 4 * g:4 * g + 4, st * 128:(st + 1) * 128], in_=src), r=[("tp", tb)], w=["XeT"])
            for fc in range(16):
                wb = fc % 2
                P.dma("pool", lambda q: q.dma_start(out=wgs[wb][:], in_=D["wg"][j].rearrange("(kc p) f -> p kc f", p=128)[:, :, fc * 128:(fc + 1) * 128]), w=[("wg", wb)])
                P.dma("pool", lambda q: q.dma_start(out=wus[wb][:], in_=D["wu"][j].rearrange("(kc p) f -> p kc f", p=128)[:, :, fc * 128:(fc + 1) * 128]), w=[("wu", wb)])
                for sgi in range(3):
                    pb = cnt % 2
                    cnt += 1
                    ssl = slice(sgi * 384, (sgi + 1) * 384)
                    P.group("pe", [mm(pgs[pb][:, 0:384], wgs[wb][:, kc, :], XeT[:, kc, ssl], kc == 0, kc == 31) for kc in range(32)], r=[("wg", wb), "XeT"], w=[("pg", pb)])
                    P.group("pe", [mm(pus[pb][:, 0:384], wus[wb][:, kc, :], XeT[:, kc, ssl], kc == 0, kc == 31) for kc in range(32)], r=[("wu", wb), "XeT"], w=[("pu", pb)])
                    P.op("act", lambda e: e.activation(out=sil[:], in_=pgs[pb][:, 0:384], func=AF.Silu), r=[("pg", pb)], w=["sil"])
                    P.op("dve", lambda e: e.tensor_tensor(out=hT[:, fc, ssl], in0=sil[:], in1=pus[pb][:, 0:384], op=ALU.mult), r=["sil", ("pu", pb)], w=["hT"])
            for nb in range(8):
                wb = nb % 2
                P.dma("pool", lambda q: q.dma_start(out=wds[wb][:], in_=D["wd"][j].rearrange("(fc p) n -> p fc n", p=128)[:, :, nb * 512:(nb + 1) * 512]), w=[("wd", wb)])
                for st in range(9):
                    pb = cnt % 2
                    cnt += 1
                    P.group("pe", [mm(pys[pb][:], hT[:, fc, st * 128:(st + 1) * 128], wds[wb][:, fc, :], fc == 0, fc == 15) for fc in range(16)], r=["hT", ("wd", wb)], w=[("py", pb)])
                    if pb == 0:
                        P.op("act", lambda e: e.copy(out=yos[pb][:], in_=pys[pb][:]), r=[("py", pb)], w=[("yo", pb)])
                    else:
                        P.op("dve", lambda e: e.tensor_copy(out=yos[pb][:], in_=pys[pb][:]), r=[("py", pb)], w=[("yo", pb)])
                    P.dma("sp", lambda q: q.dma_start(out=D["ye"][j * NS + st * 128:j * NS + (st + 1) * 128, nb * 512:(nb + 1) * 512], in_=yos[pb][:]), r=[("yo", pb)], w=["ye"])


def phase_D(P, nc, D, C):
    with ExitStack() as ES:
        acc = ES.enter_context(nc.sbuf_tensor(_u("acc"), [128, 4096], F32))
        gb0 = ES.enter_context(nc.sbuf_tensor(_u("gb0"), [128, 4096], BF16))
        gb1 = ES.enter_context(nc.sbuf_tensor(_u("gb1"), [128, 4096], BF16))
        wf = ES.enter_context(nc.sbuf_tensor(_u("wf"), [128, 4096], F32))
        junk = ES.enter_context(nc.sbuf_tensor(_u("junk"), [128, 4096], BF16))
        og = ES.enter_context(nc.sbuf_tensor(_u("og"), [128, 8], I32))
        oh = ES.enter_context(nc.sbuf_tensor(_u("oh"), [128, 8, 8], I32))
        yb = ES.enter_context(nc.sbuf_tensor(_u("yb"), [128, 16], F32))
        sl = ES.enter_context(nc.sbuf_tensor(_u("sl"), [128, 16], F32))
        af = ES.enter_context(nc.sbuf_tensor(_u("af"), [128, 16], F32))
        ix = ES.enter_context(nc.sbuf_tensor(_u("ix"), [128, 16], I32))
        sm = ES.enter_context(nc.sbuf_tensor(_u("sm"), [128, 8], F32))
        gb = [gb0, gb1]
        P.dma("sp", lambda q: q.dma_start(out=wf[:], in_=D["wfbc"]), w=["wf"])
        P.dma("sp", lambda q: q.dma_start(out=og[:], in_=D["own_g"]), w=["og"])
        P.dma("sp", lambda q: q.dma_start(out=oh[:], in_=D["own_h"]), w=["oh"])
        P.dma("sp", lambda q: q.dma_start(out=yb[:], in_=D["ybase"]), w=["yb"])
        for k in range(8):
            P.dma("pool", lambda q: q.indirect_dma_start(out=sl[:], out_offset=None, in_=D["slot_tab"], in_offset=bass.IndirectOffsetOnAxis(ap=og[:, k:k + 1], axis=0),
                                                         bounds_check=BREG(nc, NR - 1), oob_is_err=False), r=["og", "slot_tab"], w=["sl"])
            P.dma("pool", lambda q: q.indirect_dma_start(out=af[:], out_offset=None, in_=D["aff_tab"], in_offset=bass.IndirectOffsetOnAxis(ap=og[:, k:k + 1], axis=0),
                                                         bounds_check=BREG(nc, NR - 1), oob_is_err=False), r=["og", "aff_tab"], w=["af"])
            P.op("dve", lambda e: e.tensor_tensor(out=sl[:], in0=sl[:], in1=yb[:], op=ALU.add), r=["sl", "yb"], w=["sl"])
            P.op("dve", lambda e: e.tensor_copy(out=ix[:], in_=sl[:]), r=["sl"], w=["ix"])
            for r in range(8):
                P.dma("pool", lambda q: q.indirect_dma_start(out=acc[:, r * 512:(r + 1) * 512], out_offset=None, in_=D["h2_all"],
                                                             in_offset=bass.IndirectOffsetOnAxis(ap=oh[:, k, r:r + 1], axis=0),
                                                             bounds_check=BREG(nc, 8 * NR - 1), oob_is_err=False), r=["oh", "h2_all"], w=["acc"])
            for ex in range(16):
                b = ex % 2
                P.op("pool", lambda e: e.memset(gb[b][:], 0.0), w=[("gb", b)])
                P.dma("pool", lambda q: q.indirect_dma_start(out=gb[b][:], out_offset=None, in_=D["ye_all"], in_offset=bass.IndirectOffsetOnAxis(ap=ix[:, ex:ex + 1], axis=0),
                                                             bounds_check=BREG(nc, 8 * 2 * NS - 1), oob_is_err=False), r=["ix", "ye_all", ("gb", b)], w=[("gb", b)])
                P.op("dve", lambda e: e.scalar_tensor_tensor(out=acc[:], in0=gb[b][:], scalar=af[:, ex:ex + 1], in1=acc[:], op0=ALU.mult, op1=ALU.add),
                     r=[("gb", b), "af", "acc"], w=["acc"])
            P.op("act", lambda e: e.activation(out=junk[:], in_=acc[:], func=AF.Square, accum_out=sm[:, 0:1]), r=["acc"], w=["junk", "ss"])
            P.op("act", lambda e: e.activation(out=sm[:, 1:2], in_=sm[:, 0:1], func=AF.Sqrt, bias=EPS, scale=1.0 / 4096), r=["ss"], w=["rt"])
            P.op("dve", lambda e: e.reciprocal(out=sm[:, 2:3], in_=sm[:, 1:2]), r=["rt"], w=["rstd"])
            P.op("dve", lambda e: e.scalar_tensor_tensor(out=acc[:], in0=acc[:], scalar=sm[:, 2:3], in1=wf[:], op0=ALU.mult, op1=ALU.mult), r=["acc", "rstd", "wf"], w=["acc"])
            P.dma("sp", lambda q: q.dma_start(out=D["out"][k * 128:(k + 1) * 128, :], in_=acc[:]), r=["acc"], w=["out"])
import os
import ml_dtypes
from concourse.bass_utils import run_bass_kernel_spmd

BF = ml_dtypes.bfloat16
PHASES = ["A", "DNF", "DNB", "SWA", "AG1", "B1", "AG2", "B2", "AG3", "B3", "TOPK", "C", "AG4", "D"]


def build(stop_after="D", dbg=(), nt=NT, lite=False):
    nc = bass.Bass("TRN2", target_bir_lowering=False)
    D = {}

    INS = []

    def inp(name, shape, dt):
        if lite and name in ("wga", "wgb", "wa", "wb", "wo", "xcol", "wg", "wu", "wd", "w2bc", "wfbc", "wr"):
            shape = [1] * (len(shape) - 1) + [16]
        INS.append((name, list(shape), dt))
        D[name] = nc.dram_tensor(name, list(shape), dt, kind="ExternalInput").ap()

    def scr(name, shape, dt):
        if name in dbg:
            D[name] = nc.dram_tensor(name, list(shape), dt, kind="ExternalOutput").ap()
        else:
            D[name] = nc.dram_tensor(name, list(shape), dt).ap()

    inp("xp", [NR, 4096], F32); inp("w_in", [4096, CW], F32); inp("w1c", [128, 32], F32); inp("dnp", [128, 8], F32)
    inp("cwbc", [128, 3, 768], F32); inp("shm", [128, 5, 128], BF16); inp("dnmask", [2, 128, 3, 128], F32)
    inp("lastsel", [2, 128, 4], F32); inp("onwbc", [128, 128], F32); inp("swbias", [128, 3, 2, 400], F32)
    inp("swmbias", [16, 144], F32); inp("sinkbc", [128, 2], F32)
    inp("wga", [4096, 512], F32); inp("wgb", [4096, 512], F32); inp("wa", [2048, 512], F32); inp("wb", [2048, 512], F32)
    inp("wo", [4096, 512], F32); inp("xcol", [NR, 512], F32); inp("w2bc", [128, 4096], F32); inp("wr", [4096, 16], F32)
    inp("validbc", [128, NT, 16], F32); inp("lstrict", [128, 128], F32)
    inp("wg", [2, 4096, 2048], F32); inp("wu", [2, 4096, 2048], F32); inp("wd", [2, 2048, 4096], F32)
    inp("wfbc", [128, 4096], F32); inp("own_g", [128, 8], I32); inp("own_h", [128, 8, 8], I32); inp("ybase", [128, 16], F32)
    inp("identb_d", [128, 128], BF16); inp("identf_d", [128, 128], F32)
    D["out"] = nc.dram_tensor("out", [1024, 4096], F32, kind="ExternalOutput").ap()
    scr("dn_qkv", [NR, 768], BF16); scr("dn_z", [NR, 256], BF16); scr("dn_sc", [NR, 8], F32); scr("sw_qkv", [NR, 512], BF16)
    scr("o_f", [NR, 256], F32); scr("oab", [NR, 512], BF16); scr("oab_all", [8 * NR, 512], BF16)
    scr("mixed", [NR, 512], BF16); scr("mixed_all", [8 * NR, 512], BF16); scr("h2c", [NR, 512], F32); scr("h2_all", [8 * NR, 512], F32)
    scr("hn2", [NR, 4096], BF16); scr("slot_tab", [NR, 16], F32); scr("aff_tab", [NR, 16], F32)
    scr("Xe0", [NS, 4096], BF16); scr("Xe1", [NS, 4096], BF16); scr("ye", [2 * NS, 4096], BF16); scr("ye_all", [16 * NS, 4096], BF16)
    P = Prog(nc)
    last = PHASES.index(stop_after)

    def on(ph):
        return PHASES.index(ph) <= last

    with ExitStack() as ES:
        identb = ES.enter_context(nc.sbuf_tensor(_u("identb"), [128, 128], BF16))
        identf = ES.enter_context(nc.sbuf_tensor(_u("identf"), [128, 128], F32))
        onesf = ES.enter_context(nc.sbuf_tensor(_u("onesf"), [128, 128], F32))
        affs = ES.enter_context(nc.sbuf_tensor(_u("affs"), [128, NT, 16], F32))
        sloti = ES.enter_context(nc.sbuf_tensor(_u("sloti"), [128, NT, 16], I32))
        C = {"identb": identb, "identf": identf, "onesf": onesf}
        P.dma("sp", lambda q: q.dma_start(out=identb[:], in_=D["identb_d"]), w=["identb"])
        P.dma("sp", lambda q: q.dma_start(out=identf[:], in_=D["identf_d"]), w=["identf"])
        P.op("pool", lambda e: e.memset(onesf[:], 1.0), w=["onesf"])
        P.barrier()
        if on("A"):
            phase_A(P, nc, D, C, nt)
        P.barrier()
        if on("DNF"):
            dn_pass(P, nc, D, C, 0, nt)
        P.barrier()
        if on("DNB"):
            dn_pass(P, nc, D, C, 1, nt)
        P.barrier()
        if on("SWA"):
            swa_pass(P, nc, D, C, nt)
        P.barrier()
        if on("AG1"):
            allgather(P, D["oab"], D["oab_all"], ["oab"], ["oab_all"])
        P.barrier()
        if on("B1"):
            phase_B1(P, nc, D, C, nt)
        P.barrier()
        if on("AG2"):
            allgather(P, D["mixed"], D["mixed_all"], ["mixed"], ["mixed_all"])
        P.barrier()
        if on("B2"):
            phase_B2(P, nc, D, C, nt)
        P.barrier()
        if on("AG3"):
            allgather(P, D["h2c"], D["h2_all"], ["h2c"], ["h2_all"])
        P.barrier()
        if on("B3"):
            phase_B3(P, nc, D, C, affs, nt)
        P.barrier()
        if on("TOPK"):
            phase_topk(P, nc, D, C, affs, sloti, nt)
        P.barrier()
        if on("C"):
            phase_C(P, nc, D, C, sloti, nt)
        P.barrier()
        if on("AG4"):
            allgather(P, D["ye"], D["ye_all"], ["ye"], ["ye_all"])
        P.barrier()
        if on("D"):
            phase_D(P, nc, D, C)
        keys = ["out"] + list(dbg)
        P.wait_all("sp", keys)
        P.wait_all("pool", keys)
    P.INS = INS
    return nc, P


def host_inputs(x, meta_tokens, norm1_w, w_in, conv_w, a_log_fwd, a_log_bwd, dt_bias_fwd, dt_bias_bwd,
                out_norm_w, w_branch_a, attn_sink, w_branch_b, w_out, norm2_w, w_router, w_gate, w_up,
                w_down, norm_f_w, cores=range(8)):
    f32 = np.float32
    x = np.asarray(x, f32)
    xp = np.concatenate([np.zeros((112, 4096), f32), np.asarray(meta_tokens, f32), x[0]], 0)
    w_in = np.asarray(w_in, f32)[0]
    conv_w = np.asarray(conv_w, f32)[0]
    rep = lambda v, n=128: np.ascontiguousarray(np.broadcast_to(np.asarray(v, f32)[None], (n,) + np.asarray(v).shape))
    ar = np.arange(128)
    ch = ar // 64
    same = ch[:, None] == ch[None, :]
    lt = ar[:, None] < ar[None, :]
    le = ar[:, None] <= ar[None, :]
    dnmask = np.zeros((2, 128, 3, 128), f32)
    dnmask[0, :, 0, :] = same & le
    dnmask[0, :, 1, :] = same & lt.T
    dnmask[0, :, 2, :] = same & le
    dnmask[1, :, 0, :] = same & le.T
    dnmask[1, :, 1, :] = same & lt
    dnmask[1, :, 2, :] = same & le.T
    lastsel = np.zeros((2, 128, 4), f32)
    lastsel[:, 0:64, 2] = 1; lastsel[:, 64:128, 3] = 1
    lastsel[0, 63, 0] = 1; lastsel[0, 127, 1] = 1; lastsel[1, 0, 0] = 1; lastsel[1, 64, 1] = 1
    shm = np.zeros((128, 5, 128), f32)
    for t in range(1, 128):
        shm[t - 1, 0, t] = 1
        shm[t, 1, t - 1] = 1
    shm[127, 2, 0] = 1
    shm[0, 3, 127] = 1
    shm = shm.astype(BF)
    q_i = np.arange(128)[:, None]
    kk = np.arange(384)[None, :]
    dist = 128 + q_i - kk
    okb = np.abs(dist) <= 128
    slopes = 2.0 ** (-8.0 * np.arange(1, 17, dtype=np.float64) / 16)
    mb = np.zeros((16, 144), f32)
    jj = np.arange(128)[None, :]
    ii = np.arange(16)[:, None]
    mb[:, 0:128] = np.where(jj <= 112 + ii, 0.0, -30000.0)
    validbc = np.ones((128, NT, 16), f32)
    validbc[0:112, 0, :] = 0
    lstrict = (ar[:, None] < ar[None, :]).astype(f32)
    common = dict(xp=xp, shm=shm, dnmask=dnmask, lastsel=lastsel, onwbc=rep(np.asarray(out_norm_w, f32)[0]),
                  swmbias=mb, w1c=np.ascontiguousarray(np.asarray(norm1_w, f32)[0].reshape(32, 128).T),
                  w2bc=rep(np.asarray(norm2_w, f32)[0]), wfbc=rep(np.asarray(norm_f_w, f32)), validbc=validbc, lstrict=lstrict,
                  identb_d=np.eye(128, dtype=f32).astype(BF), identf_d=np.eye(128, dtype=f32))
    wa_full = np.asarray(w_branch_a, f32)[0]; wb_full = np.asarray(w_branch_b, f32)[0]; wo_full = np.asarray(w_out, f32)[0]
    wr_full = np.asarray(w_router, f32)[0]
    alf = np.asarray(a_log_fwd, f32)[0]; alb = np.asarray(a_log_bwd, f32)[0]
    dbf = np.asarray(dt_bias_fwd, f32)[0]; dbb = np.asarray(dt_bias_bwd, f32)[0]
    sink = np.asarray(attn_sink, f32)[0]
    maps = []
    for c in cores:
        hs = [2 * c, 2 * c + 1]
        kv = c // 2
        cols = []
        for base in (0, 2048, 4096, 6144):
            for h in hs:
                cols.append(np.arange(base + h * 128, base + (h + 1) * 128))
        for h in hs:
            cols.append(np.arange(8256 + h * 128, 8256 + (h + 1) * 128))
        cols.append(np.arange(10304 + kv * 128, 10304 + (kv + 1) * 128))
        cols.append(np.arange(10816 + kv * 128, 10816 + (kv + 1) * 128))
        for base in (8192, 8208, 8224, 8240):
            cols.append(np.array([base + hs[0], base + hs[1]]))
        cols = np.concatenate(cols)
        assert cols.shape[0] == CW
        ccols = np.concatenate([np.arange(base + h * 128, base + (h + 1) * 128) for base in (0, 2048, 4096) for h in hs])
        bias = np.zeros((128, 3, 2, 400), f32)
        for hl, h in enumerate(hs):
            for var in range(3):
                ok = okb.copy()
                if var == 0:
                    ok = ok & (kk >= 128)
                if var == 2:
                    ok = ok & (kk < 256)
                bias[:, var, hl, 0:384] = np.where(ok, -slopes[h] * np.abs(dist), -30000.0)
        perm = hs + [e for e in range(16) if e not in hs]
        og = ((1 + 8 * c + np.arange(8))[None, :] * 128 + ar[:, None]).astype(np.int32)
        oh = (np.arange(8)[None, None, :] * NR + og[:, :, None]).astype(np.int32)
        yb = np.array([(e // 2) * 2 * NS + (e % 2) * NS for e in perm], f32)
        m = dict(common)
        m.update(w_in=np.ascontiguousarray(w_in[:, cols]),
                 dnp=rep(np.array([alf[hs[0]], alf[hs[1]], alb[hs[0]], alb[hs[1]], dbf[hs[0]], dbf[hs[1]], dbb[hs[0]], dbb[hs[1]]], f32)),
                 cwbc=rep(np.ascontiguousarray(conv_w[:, ccols])), swbias=bias, sinkbc=rep(np.array([sink[hs[0]], sink[hs[1]]], f32)),
                 wga=np.ascontiguousarray(w_in[:, 11328 + 512 * c:11328 + 512 * (c + 1)]),
                 wgb=np.ascontiguousarray(w_in[:, 15424 + 512 * c:15424 + 512 * (c + 1)]),
                 wa=np.ascontiguousarray(wa_full[:, 512 * c:512 * (c + 1)]), wb=np.ascontiguousarray(wb_full[:, 512 * c:512 * (c + 1)]),
                 wo=np.ascontiguousarray(wo_full[:, 512 * c:512 * (c + 1)]), xcol=np.ascontiguousarray(xp[:, 512 * c:512 * (c + 1)]),
                 wr=np.ascontiguousarray(wr_full[:, perm]),
                 wg=np.ascontiguousarray(np.asarray(w_gate)[0, 2 * c:2 * c + 2], dtype=f32), wu=np.ascontiguousarray(np.asarray(w_up)[0, 2 * c:2 * c + 2], dtype=f32),
                 wd=np.ascontiguousarray(np.asarray(w_down)[0, 2 * c:2 * c + 2], dtype=f32),
                 own_g=og, own_h=oh, ybase=rep(yb))
        maps.append(m)
    return maps


def kernel(**inputs):
    maps = host_inputs(**inputs)
    nc, _ = build()
    res = run_bass_kernel_spmd(nc, maps, core_ids=list(range(8)))
    out = np.concatenate([np.asarray(res.results[c]["out"], np.float32) for c in range(8)], 0)
    return out.reshape(1, 8192, 4096)
```

```python
from contextlib import ExitStack
_UC = [0]
def _u(n):
    _UC[0] += 1
    return f'{n}_{_UC[0]}'
import numpy as np
import concourse.bass as bass
import concourse.mybir as mybir

F32 = mybir.dt.float32
BF16 = mybir.dt.bfloat16
I32 = mybir.dt.int32
U32 = mybir.dt.uint32
AF = mybir.ActivationFunctionType
ALU = mybir.AluOpType
AX = mybir.AxisListType

EPOCH = 8000
NDMA = 24


class Prog:
    def __init__(self, nc):
        self.nc = nc
        self.engs = {"pe": nc.tensor, "dve": nc.vector, "act": nc.scalar,
                     "pool": nc.gpsimd, "sp": nc.sync}
        self.sem = {}
        self.cnt = {}
        self.nsem = 0
        for e in self.engs:
            self._new_epoch(e)
        self.dsem = [nc.alloc_semaphore(name=f"dq{i}") for i in range(NDMA)]
        self.dcnt = [0] * NDMA
        self.drr = 0
        self.last_w = {}
        self.readers = {}
        self.seen = {e: {} for e in self.engs}
        self.semobj = {}
        self.ninst = 0
        self.kp = None
        self.shared = set()

    def _new_epoch(self, e):
        s = self.nc.alloc_semaphore(name=f"e_{e}_{self.nsem}")
        self.nsem += 1
        self.sem[e] = s
        self.cnt[e] = 0

    def _wait(self, e, tok):
        s, v = tok
        key = id(s)
        self.semobj[key] = s
        if self.seen[e].get(key, 0) >= v:
            return
        self.engs[e].wait_ge(s, v)
        self.seen[e][key] = v

    def _k(self, k):
        if self.kp is None:
            return k
        root = k[0] if isinstance(k, tuple) else k
        return k if root in self.shared else (self.kp, k)

    def _deps(self, r, w):
        r = [self._k(k) for k in r]
        w = [self._k(k) for k in w]
        toks = {}

        def add(tok):
            if tok is None:
                return
            k = id(tok[0])
            if k not in toks or toks[k][1] < tok[1]:
                toks[k] = tok

        for k in r:
            add(self.last_w.get(k))
        for k in w:
            add(self.last_w.get(k))
            for t in self.readers.get(k, {}).values():
                add(t)
        return list(toks.values())

    def _register(self, tok, r, w):
        r = [self._k(k) for k in r]
        w = [self._k(k) for k in w]
        for k in r:
            d = self.readers.setdefault(k, {})
            d[id(tok[0])] = tok
        for k in w:
            self.last_w[k] = tok
            self.readers[k] = {}

    def op(self, e, fn, r=(), w=()):
        for tok in self._deps(r, w):
            self._wait(e, tok)
        if self.cnt[e] >= EPOCH:
            self._new_epoch(e)
        inst = fn(self.engs[e])
        self.cnt[e] += 1
        inst.then_inc(self.sem[e], 1)
        tok = (self.sem[e], self.cnt[e])
        self._register(tok, r, w)
        self.ninst += 1
        return tok

    def group(self, e, fns, r=(), w=()):
        for tok in self._deps(r, w):
            self._wait(e, tok)
        if self.cnt[e] + len(fns) >= EPOCH:
            self._new_epoch(e)
        tok = None
        for fn in fns:
            inst = fn(self.engs[e])
            self.cnt[e] += 1
            inst.then_inc(self.sem[e], 1)
            tok = (self.sem[e], self.cnt[e])
            self.ninst += 1
        self._register(tok, r, w)
        return tok

    def dma(self, q, fn, r=(), w=(), inc=16):
        i = self.drr
        self.drr = (i + 1) % NDMA
        if self.dcnt[i] > 0:
            self._wait(q, (self.dsem[i], self.dcnt[i]))
        for tok in self._deps(r, w):
            self._wait(q, tok)
        inst = fn(self.engs[q])
        self.dcnt[i] += inc
        inst.then_inc(self.dsem[i], inc)
        tok = (self.dsem[i], self.dcnt[i])
        self._register(tok, r, w)
        self.ninst += 1
        return tok

    def wait_all(self, e, keys):
        for k in keys:
            t = self.last_w.get(k)
            if t is not None:
                self._wait(e, t)

    def barrier(self):
        toks = [(self.sem[f], self.cnt[f]) for f in self.engs if self.cnt[f] > 0]
        toks += [(self.dsem[i], self.dcnt[i]) for i in range(NDMA) if self.dcnt[i] > 0]
        for e in self.engs:
            for tok in toks:
                self._wait(e, tok)
NT = 65
NR = NT * 128
CW = 1544
EPS = 1e-6


def mm(ps, lhsT, rhs, start=True, stop=True):
    return lambda e: e.matmul(ps, lhsT=lhsT, rhs=rhs, start=start, stop=stop)


def phase_A(P, nc, D, C, nt=NT):
    with ExitStack() as ES:
        W = ES.enter_context(nc.sbuf_tensor(_u("W"), [128, 32, CW], BF16))
        xt0 = ES.enter_context(nc.sbuf_tensor(_u("xt0"), [128, 4096], F32))
        xt1 = ES.enter_context(nc.sbuf_tensor(_u("xt1"), [128, 4096], F32))
        xn = ES.enter_context(nc.sbuf_tensor(_u("xn"), [128, 4096], BF16))
        xT = ES.enter_context(nc.sbuf_tensor(_u("xT"), [128, 32, 128], BF16))
        proj = ES.enter_context(nc.sbuf_tensor(_u("proj"), [128, CW], F32))
        rr = ES.enter_context(nc.sbuf_tensor(_u("rr"), [128, 9, 768], BF16))
        cw = ES.enter_context(nc.sbuf_tensor(_u("cw"), [128, 3, 768], F32))
        qs = ES.enter_context(nc.sbuf_tensor(_u("qs"), [128, 768], F32))
        qkvb = ES.enter_context(nc.sbuf_tensor(_u("qkvb"), [128, 768], BF16))
        zsb = ES.enter_context(nc.sbuf_tensor(_u("zsb"), [128, 256], BF16))
        swb = ES.enter_context(nc.sbuf_tensor(_u("swb"), [128, 512], BF16))
        junk = ES.enter_context(nc.sbuf_tensor(_u("junk"), [128, 128], BF16))
        sm = ES.enter_context(nc.sbuf_tensor(_u("sm"), [128, 64], F32))
        w1c = ES.enter_context(nc.sbuf_tensor(_u("w1c"), [128, 32], F32))
        dnp = ES.enter_context(nc.sbuf_tensor(_u("dnp"), [128, 8], F32))
        shm = ES.enter_context(nc.sbuf_tensor(_u("shm"), [128, 5, 128], BF16))
        tp0 = ES.enter_context(nc.psum_tensor(_u("tp0"), [128, 512], BF16))
        tp1 = ES.enter_context(nc.psum_tensor(_u("tp1"), [128, 512], BF16))
        pj0 = ES.enter_context(nc.psum_tensor(_u("pj0"), [128, 512], F32))
        pj1 = ES.enter_context(nc.psum_tensor(_u("pj1"), [128, 512], F32))
        pj2 = ES.enter_context(nc.psum_tensor(_u("pj2"), [128, 512], F32))
        pj3 = ES.enter_context(nc.psum_tensor(_u("pj3"), [128, 512], F32))
        cv0 = ES.enter_context(nc.psum_tensor(_u("cv0"), [128, 512], F32))
        cv1 = ES.enter_context(nc.psum_tensor(_u("cv1"), [128, 512], F32))
        xt = [xt0, xt1]
        tp = [tp0, tp1]
        pj = [pj0, pj1, pj2, pj3]
        P.dma("sp", lambda q: q.dma_start(out=w1c[:], in_=D["w1c"]), w=["w1c"])
        P.dma("sp", lambda q: q.dma_start(out=dnp[:], in_=D["dnp"]), w=["dnp"])
        P.dma("sp", lambda q: q.dma_start(out=cw[:], in_=D["cwbc"]), w=["cw"])
        P.dma("sp", lambda q: q.dma_start(out=shm[:], in_=D["shm"]), w=["shm"])
        P.op("act", lambda e: e.activation(out=sm[:, 16:20], in_=dnp[:, 0:4], func=AF.Exp), r=["dnp"], w=["negA"])
        P.op("dve", lambda e: e.tensor_scalar(out=sm[:, 16:20], in0=sm[:, 16:20], scalar1=-1.0, scalar2=None, op0=ALU.mult), r=["negA"], w=["negA"])
        for kc in range(32):
            b = kc % 2
            P.dma("sp", lambda q, kc=kc, b=b: q.dma_start(out=xt[b][:, 0:CW], in_=D["w_in"][kc * 128:(kc + 1) * 128, :]), w=[("xt", b)])
            P.op("act", lambda e, kc=kc, b=b: e.activation(out=W[:, kc, :], in_=xt[b][:, 0:CW], func=AF.Copy, scale=w1c[:, kc:kc + 1]),
                 r=[("xt", b), "w1c"], w=["W"])
        ident = C["identb"]

        def conv_tile(t):
            s, sp_, sn = (t % 3) * 3, ((t - 1) % 3) * 3, ((t + 1) % 3) * 3
            for half, (c0, c1, cv) in enumerate([(0, 512, cv0), (512, 768, cv1)]):
                fns = []
                ops = [(shm[:, 0, :], rr[:, s + 0, c0:c1]), (ident[:], rr[:, s + 1, c0:c1]), (shm[:, 1, :], rr[:, s + 2, c0:c1])]
                if t > 0:
                    ops.append((shm[:, 2, :], rr[:, sp_ + 0, c0:c1]))
                if t < nt - 1:
                    ops.append((shm[:, 3, :], rr[:, sn + 2, c0:c1]))
                for i, (l, r_) in enumerate(ops):
                    fns.append(mm(cv[:, 0:c1 - c0], l, r_, start=(i == 0), stop=(i == len(ops) - 1)))
                P.group("pe", fns, r=[("rr", t % 3), ("rr", (t - 1) % 3), ("rr", (t + 1) % 3), "shm", "identb"], w=[("cv", half)])
                P.op("act", lambda e, c0=c0, c1=c1, cv=cv: e.activation(out=qs[:, c0:c1], in_=cv[:, 0:c1 - c0], func=AF.Silu),
                     r=[("cv", half)], w=[("qs", half)])
            for i in range(4):
                P.op("act", lambda e, i=i: e.activation(out=junk[:], in_=qs[:, i * 128:(i + 1) * 128], func=AF.Square, accum_out=sm[:, 4 + i:5 + i]),
                     r=[("qs", 0), ("qs", 1)], w=["junk", "ss4"])
            P.op("act", lambda e: e.activation(out=sm[:, 8:12], in_=sm[:, 4:8], func=AF.Sqrt, bias=EPS, scale=1.0), r=["ss4"], w=["rt4"])
            P.op("dve", lambda e: e.reciprocal(out=sm[:, 12:16], in_=sm[:, 8:12]), r=["rt4"], w=["r4"])
            P.op("dve", lambda e: e.tensor_scalar(out=sm[:, 12:14], in0=sm[:, 12:14], scalar1=float(128 ** -0.5), scalar2=None, op0=ALU.mult), r=["r4"], w=["r4"])
            for i in range(4):
                P.op("dve", lambda e, i=i: e.tensor_scalar(out=qkvb[:, i * 128:(i + 1) * 128], in0=qs[:, i * 128:(i + 1) * 128],
                                                          scalar1=sm[:, 12 + i:13 + i], scalar2=None, op0=ALU.mult),
                     r=[("qs", 0), ("qs", 1), "r4"], w=["qkvb"])
            P.op("pool", lambda e: e.tensor_copy(out=qkvb[:, 512:768], in_=qs[:, 512:768]), r=[("qs", 0), ("qs", 1)], w=["qkvb"])
            if t == 0:
                P.op("pool", lambda e: e.memset(qkvb[0:112, :], 0.0), r=["qkvb"], w=["qkvb"])
            P.dma("sp", lambda q, t=t: q.dma_start(out=D["dn_qkv"][t * 128:(t + 1) * 128, :], in_=qkvb[:]), r=["qkvb"], w=["dn_qkv"])

        for t in range(nt):
            b = t % 2
            P.dma("sp", lambda q, t=t, b=b: q.dma_start(out=xt[b][:], in_=D["xp"][t * 128:(t + 1) * 128, :]), w=[("xt", b)])
            P.op("act", lambda e, b=b: e.activation(out=xn[:], in_=xt[b][:], func=AF.Square, accum_out=sm[:, 0:1]), r=[("xt", b)], w=["xn", "ss"])
            P.op("act", lambda e: e.activation(out=sm[:, 1:2], in_=sm[:, 0:1], func=AF.Sqrt, bias=EPS, scale=1.0 / 4096), r=["ss"], w=["rt"])
            P.op("dve", lambda e: e.reciprocal(out=sm[:, 2:3], in_=sm[:, 1:2]), r=["rt"], w=["rstd"])
            P.op("act", lambda e, b=b: e.activation(out=xn[:], in_=xt[b][:], func=AF.Copy, scale=sm[:, 2:3]), r=[("xt", b), "rstd"], w=["xn"])
            for g in range(8):
                tb = g % 2
                P.group("pe", [(lambda e, g=g, j=j, tb=tb: e.transpose(tp[tb][:, j * 128:(j + 1) * 128], xn[:, (4 * g + j) * 128:(4 * g + j + 1) * 128], ident[:]))
                               for j in range(4)], r=["xn", "identb"], w=[("tp", tb)])
                eng = "act" if g % 2 == 0 else "dve"
                if eng == "act":
                    P.op("act", lambda e, g=g, tb=tb: e.copy(out=xT[:, 4 * g:4 * g + 4, :], in_=tp[tb][:].rearrange("p (a b) -> p a b", b=128)), r=[("tp", tb)], w=["xT"])
                else:
                    P.op("dve", lambda e, g=g, tb=tb: e.tensor_copy(out=xT[:, 4 * g:4 * g + 4, :], in_=tp[tb][:].rearrange("p (a b) -> p a b", b=128)), r=[("tp", tb)], w=["xT"])
            P.dma("pool", lambda q, t=t: q.dma_start(out=D["xT_s"][t * 128:(t + 1) * 128, :], in_=xT[:].rearrange("p a b -> p (a b)")), r=["xT"], w=["xT_s"])
            for cg in range(4):
                c0 = cg * 512
                c1 = min(CW, c0 + 512)
                P.group("pe", [mm(pj[cg][:, 0:c1 - c0], xT[:, kc, :], W[:, kc, c0:c1], start=(kc == 0), stop=(kc == 31)) for kc in range(32)],
                        r=["xT", "W"], w=[("pj", cg)])
                P.op("act" if cg % 2 == 0 else "dve",
                     (lambda e, cg=cg, c0=c0, c1=c1: e.copy(out=proj[:, c0:c1], in_=pj[cg][:, 0:c1 - c0])) if cg % 2 == 0 else
                     (lambda e, cg=cg, c0=c0, c1=c1: e.tensor_copy(out=proj[:, c0:c1], in_=pj[cg][:, 0:c1 - c0])),
                     r=[("pj", cg)], w=["proj"])
            s = (t % 3) * 3
            for i in range(3):
                P.op("pool", lambda e, i=i, s=s: e.tensor_tensor(out=rr[:, s + i, :], in0=proj[:, 0:768], in1=cw[:, i, :], op=ALU.mult),
                     r=["proj", "cw"], w=[("rr", t % 3)])
            P.op("act", lambda e: e.activation(out=zsb[:], in_=proj[:, 768:1024], func=AF.Silu), r=["proj"], w=["zsb"])
            P.dma("sp", lambda q, t=t: q.dma_start(out=D["dn_z"][t * 128:(t + 1) * 128, :], in_=zsb[:]), r=["zsb"], w=["dn_z"])
            P.op("act", lambda e: e.activation(out=sm[:, 40:44], in_=proj[:, 1536:1540], func=AF.Sigmoid), r=["proj"], w=["sc"])
            P.op("dve", lambda e: e.tensor_tensor(out=sm[:, 20:24], in0=proj[:, 1540:1544], in1=dnp[:, 4:8], op=ALU.add), r=["proj", "dnp"], w=["spx"])
            P.op("act", lambda e: e.activation(out=sm[:, 24:28], in_=sm[:, 20:24], func=AF.Abs), r=["spx"], w=["spa"])
            P.op("act", lambda e: e.activation(out=sm[:, 28:32], in_=sm[:, 24:28], func=AF.Exp, scale=-1.0), r=["spa"], w=["spe"])
            P.op("act", lambda e: e.activation(out=sm[:, 32:36], in_=sm[:, 28:32], func=AF.Ln, bias=1.0, scale=1.0), r=["spe"], w=["spl"])
            P.op("dve", lambda e: e.tensor_scalar(out=sm[:, 36:40], in0=sm[:, 20:24], scalar1=0.0, scalar2=None, op0=ALU.max), r=["spx"], w=["spm"])
            P.op("dve", lambda e: e.tensor_tensor(out=sm[:, 36:40], in0=sm[:, 36:40], in1=sm[:, 32:36], op=ALU.add), r=["spm", "spl"], w=["spm"])
            P.op("dve", lambda e: e.tensor_tensor(out=sm[:, 44:48], in0=sm[:, 36:40], in1=sm[:, 16:20], op=ALU.mult), r=["spm", "negA"], w=["sc"])
            P.dma("sp", lambda q, t=t: q.dma_start(out=D["dn_sc"][t * 128:(t + 1) * 128, :], in_=sm[:, 40:48]), r=["sc"], w=["dn_sc"])
            P.op("pool", lambda e: e.tensor_copy(out=swb[:], in_=proj[:, 1024:1536]), r=["proj"], w=["swb"])
            P.dma("sp", lambda q, t=t: q.dma_start(out=D["sw_qkv"][t * 128:(t + 1) * 128, :], in_=swb[:]), r=["swb"], w=["sw_qkv"])
            if t > 0:
                conv_tile(t - 1)
        conv_tile(nt - 1)
def dn_pass(P, nc, D, C, d, nt=NT):
    with ExitStack() as ES:
        qk0 = ES.enter_context(nc.sbuf_tensor(_u("qk0"), [128, 768], BF16))
        qk1 = ES.enter_context(nc.sbuf_tensor(_u("qk1"), [128, 768], BF16))
        sc0 = ES.enter_context(nc.sbuf_tensor(_u("sc0"), [128, 8], F32))
        sc1 = ES.enter_context(nc.sbuf_tensor(_u("sc1"), [128, 8], F32))
        kqT_ = ES.enter_context(nc.sbuf_tensor(_u("kqT"), [128, 2, 2, 128], BF16))
        fa_ = ES.enter_context(nc.sbuf_tensor(_u("f32a"), [128, 2, 3, 128], F32))
        fb_ = ES.enter_context(nc.sbuf_tensor(_u("fbb"), [128, 2, 5, 128], BF16))
        ba_ = ES.enter_context(nc.sbuf_tensor(_u("bfa"), [128, 2, 11, 128], BF16))
        Sb = ES.enter_context(nc.sbuf_tensor(_u("Sb"), [128, 2, 128], BF16))
        ds_ = ES.enter_context(nc.sbuf_tensor(_u("dsm"), [128, 2, 32], F32))
        osb_ = ES.enter_context(nc.sbuf_tensor(_u("osb"), [64, 2, 2, 128], F32))
        ofl_ = ES.enter_context(nc.sbuf_tensor(_u("ofl"), [64, 2, 2, 128], F32))
        zl_ = ES.enter_context(nc.sbuf_tensor(_u("zl"), [64, 2, 2, 128], BF16))
        oout_ = ES.enter_context(nc.sbuf_tensor(_u("oout"), [64, 2, 2, 128], BF16))
        msk = ES.enter_context(nc.sbuf_tensor(_u("msk"), [128, 3, 128], F32))
        lsel = ES.enter_context(nc.sbuf_tensor(_u("lsel"), [128, 4], F32))
        onw = ES.enter_context(nc.sbuf_tensor(_u("onw"), [128, 128], F32))
        xa0 = ES.enter_context(nc.psum_tensor(_u("xa0"), [128, 512], F32))
        xb0 = ES.enter_context(nc.psum_tensor(_u("xb0"), [128, 512], F32))
        xc0 = ES.enter_context(nc.psum_tensor(_u("xc0"), [128, 512], F32))
        xa1 = ES.enter_context(nc.psum_tensor(_u("xa1"), [128, 512], F32))
        xb1 = ES.enter_context(nc.psum_tensor(_u("xb1"), [128, 512], F32))
        xc1 = ES.enter_context(nc.psum_tensor(_u("xc1"), [128, 512], F32))
        xd0 = ES.enter_context(nc.psum_tensor(_u("xd0"), [128, 512], F32))
        xd1 = ES.enter_context(nc.psum_tensor(_u("xd1"), [128, 512], F32))
        qk = [qk0, qk1]
        scs = [sc0, sc1]
        XA, XB, XC, XD = [xa0, xa1], [xb0, xb1], [xc0, xc1], [xd0, xd1]
        identb, identf, onesf = C["identb"], C["identf"], C["onesf"]
        P.shared = {"qk", "sc", "identb", "identf", "onesf", "msk", "lsel", "onw", "dn_qkv", "dn_sc", "dn_z", "o_f", "oab", "S"}
        P.dma("sp", lambda q: q.dma_start(out=msk[:], in_=D["dnmask"][d]), w=["msk"])
        P.dma("sp", lambda q: q.dma_start(out=lsel[:], in_=D["lastsel"][d]), w=["lsel"])
        P.dma("sp", lambda q: q.dma_start(out=onw[:], in_=D["onwbc"]), w=["onw"])
        for h in range(2):
            P.op("pool", lambda e, h=h: e.memset(Sb[:, h, :], 0.0), w=[("S", h)])

        def unit(h, t, b):
            kqT, fa, ba, ds, fb = kqT_[:, h], fa_[:, h], ba_[:, h], ds_[:, h], fb_[:, h]
            osb, ofl, zl, oout = osb_[:, h], ofl_[:, h], zl_[:, h], oout_[:, h]
            pA = XA[h]
            pB, pD = XB[h][:, 0:128], XB[h][:, 128:256]
            pC, pE = XC[h][:, 0:128], XC[h][:, 128:256]
            pF, pG = XD[h][:, 0:128], XD[h][:, 128:256]
            pT = XA[h][:, 256:384].bitcast(BF16)
            pDb = XB[h][:, 128:256].bitcast(BF16)[:, 0:128]
            qn = qk[b][:, h * 128:(h + 1) * 128]
            kn = qk[b][:, 256 + h * 128:256 + (h + 1) * 128]
            vv = qk[b][:, 512 + h * 128:512 + (h + 1) * 128]
            beta = scs[b][:, d * 2 + h:d * 2 + h + 1]
            g = scs[b][:, 4 + d * 2 + h:4 + d * 2 + h + 1]
            rq = [("qk", b), ("sc", b)]
            yield P.group("pe", [lambda e: e.transpose(pT[:, 0:128], kn, identb[:]), lambda e: e.transpose(pT[:, 128:256], qn, identb[:])],
                          r=rq + ["identb"], w=["pT"])
            yield P.op("act", lambda e: e.copy(out=kqT, in_=pT[:, 0:256].rearrange("p (a b) -> p a b", b=128)), r=["pT"], w=["kqT"])
            yield P.op("dve", lambda e: e.tensor_scalar(out=fa[:, 0, :], in0=onesf[:], scalar1=g, scalar2=None, op0=ALU.mult), r=rq + ["onesf"], w=["gB"])
            yield P.group("pe", [mm(pA[:, 0:128], fa[:, 0, :], msk[:, 0, :]), mm(pA[:, 128:129], msk[:, 0, :], g)], r=["gB", "msk", "kqT"] + rq, w=["pA"])
            yield P.op("act", lambda e: e.copy(out=ds[:, 0:1], in_=pA[:, 128:129]), r=["pA"], w=["gc"])
            yield P.op("dve", lambda e: e.tensor_scalar(out=fa[:, 1, :], in0=pA[:, 0:128], scalar1=ds[:, 0:1], scalar2=0.0, op0=ALU.subtract, op1=ALU.max), r=["pA", "gc"], w=["dec"])
            yield P.op("dve", lambda e: e.tensor_scalar(out=fa[:, 2, :], in0=pA[:, 0:128], scalar1=ds[:, 0:1], scalar2=0.0, op0=ALU.subtract, op1=ALU.min), r=["pA", "gc"], w=["decT"])
            yield P.op("act", lambda e: e.activation(out=fa[:, 1, :], in_=fa[:, 1, :], func=AF.Exp, scale=-1.0), r=["dec"], w=["dec"])
            yield P.op("act", lambda e: e.activation(out=fa[:, 2, :], in_=fa[:, 2, :], func=AF.Exp), r=["decT"], w=["decT"])
            yield P.op("dve", lambda e: e.tensor_tensor(out=fa[:, 1, :], in0=fa[:, 1, :], in1=msk[:, 1, :], op=ALU.mult), r=["dec", "msk"], w=["dec"])
            yield P.op("pool", lambda e: e.tensor_tensor(out=fa[:, 2, :], in0=fa[:, 2, :], in1=msk[:, 2, :], op=ALU.mult), r=["decT", "msk"], w=["decT"])
            yield P.op("pe", mm(pB, kqT[:, 0, :], kqT[:, 0, :]), r=["kqT"], w=["pB"])
            yield P.op("pe", mm(pC, kqT[:, 0, :], kqT[:, 1, :]), r=["kqT"], w=["pC"])
            yield P.op("dve", lambda e: e.tensor_scalar(out=ds[:, 1:2], in0=beta, scalar1=-1.0, scalar2=None, op0=ALU.mult), r=rq, w=["nbeta"])
            yield P.op("dve", lambda e: e.scalar_tensor_tensor(out=fb[:, 0, :], in0=pB, scalar=ds[:, 1:2], in1=fa[:, 1, :], op0=ALU.mult, op1=ALU.mult),
                       r=["pB", "nbeta", "dec"], w=["R"])
            yield P.op("dve", lambda e: e.tensor_tensor(out=ba[:, 0, :], in0=pC, in1=fa[:, 2, :], op=ALU.mult), r=["pC", "decT"], w=["AT"])
            yield P.op("pe", lambda e: e.transpose(pDb, fb[:, 0, :], identb[:]), r=["R", "identb"], w=["pD"])
            yield P.op("act", lambda e: e.copy(out=fb[:, 1, :], in_=pDb), r=["pD"], w=["Pm"])
            yield P.op("dve", lambda e: e.tensor_tensor(out=fb[:, 2, :], in0=fb[:, 1, :], in1=identb[:], op=ALU.add), r=["Pm", "identb"], w=["TT"])
            Rk, Pk = 0, 1
            for k in range(1, 6):
                Rn, Pn = (3, 4) if Rk == 0 else (0, 1)
                yield P.op("pe", mm(pD, fb[:, Pk, :], fb[:, Rk, :]), r=["R", "Pm", "R2", "P2"], w=["pD"])
                yield P.op("act", lambda e: e.copy(out=fb[:, Rn, :], in_=pD), r=["pD"], w=["R2" if Rn == 3 else "R"])
                if k < 5:
                    yield P.op("pe", mm(pE, fb[:, Rk, :], fb[:, Pk, :]), r=["R", "Pm", "R2", "P2", "AT"], w=["pE"])
                    yield P.op("dve", lambda e: e.tensor_copy(out=fb[:, Pn, :], in_=pE), r=["pE"], w=["P2" if Pn == 4 else "Pm"])
                yield P.op("pe", mm(pF, fb[:, Rn, :], fb[:, 2, :]), r=["R", "R2", "TT"], w=["pF"])
                yield P.op("dve", lambda e: e.tensor_tensor(out=fb[:, 2, :], in0=fb[:, 2, :], in1=pF, op=ALU.add), r=["pF", "TT"], w=["TT"])
                Rk, Pk = Rn, Pn
            yield P.op("act", lambda e: e.activation(out=ds[:, 2:3], in_=ds[:, 0:1], func=AF.Exp), r=["gc"], w=["egc"])
            yield P.op("dve", lambda e: e.tensor_tensor(out=ds[:, 3:4], in0=ds[:, 2:3], in1=beta, op=ALU.mult), r=["egc"] + rq, w=["bk"])
            yield P.op("act", lambda e: e.activation(out=ba[:, 2, :], in_=vv, func=AF.Copy, scale=beta), r=rq, w=["vb"])
            yield P.op("act", lambda e: e.activation(out=ba[:, 3, :], in_=kn, func=AF.Copy, scale=ds[:, 3:4]), r=rq + ["bk"], w=["kbg"])
            yield P.op("pe", mm(pB, fb[:, 2, :], ba[:, 2, :]), r=["TT", "vb"], w=["pB"])
            yield P.op("pe", mm(pC, fb[:, 2, :], ba[:, 3, :]), r=["TT", "kbg"], w=["pC"])
            yield P.op("act", lambda e: e.copy(out=ba[:, 4, :], in_=pB), r=["pB"], w=["u"])
            yield P.op("act", lambda e: e.activation(out=ba[:, 5, :], in_=pC, func=AF.Copy, scale=-1.0), r=["pC"], w=["wneg"])
            yield P.op("dve", lambda e: e.tensor_scalar(out=ds[:, 4:6], in0=lsel[:, 0:2], scalar1=ds[:, 0:1], scalar2=None, op0=ALU.mult), r=["lsel", "gc"], w=["gsel"])
            yield P.op("pe", mm(pA[:, 0:2], onesf[:], ds[:, 4:6]), r=["onesf", "gsel"], w=["pA"])
            yield P.op("act", lambda e: e.copy(out=ds[:, 6:8], in_=pA[:, 0:2]), r=["pA"], w=["glb"])
            yield P.op("act", lambda e: e.activation(out=ds[:, 8:10], in_=ds[:, 6:8], func=AF.Exp), r=["glb"], w=["egl"])
            yield P.op("dve", lambda e: e.tensor_copy(out=ds[0:64, 10:11], in_=ds[0:64, 6:7]), r=["glb"], w=["glc"])
            yield P.op("dve", lambda e: e.tensor_copy(out=ds[64:128, 10:11], in_=ds[64:128, 7:8]), r=["glb"], w=["glc"])
            yield P.op("act", lambda e: e.activation(out=ds[:, 11:12], in_=ds[:, 0:1], func=AF.Exp, scale=-1.0, bias=ds[:, 10:11]), r=["gc", "glc"], w=["kds"])
            yield P.op("dve", lambda e: e.tensor_scalar(out=ds[:, 20:22], in0=lsel[:, 2:4], scalar1=ds[:, 11:12], scalar2=None, op0=ALU.mult), r=["lsel", "kds"], w=["kds2"])
            yield P.op("dve", lambda e: e.tensor_scalar(out=ba[:, 6, :], in0=kn, scalar1=ds[:, 20:21], scalar2=None, op0=ALU.mult), r=rq + ["kds2"], w=["kdec"])
            yield P.op("pool", lambda e: e.tensor_scalar(out=ba[:, 10, :], in0=kn, scalar1=ds[:, 21:22], scalar2=None, op0=ALU.mult), r=rq + ["kds2"], w=["kdec"])
            yield P.op("act", lambda e: e.activation(out=ba[:, 7, :], in_=identb[:], func=AF.Copy, scale=ds[:, 2:3]), r=["identb", "egc"], w=["Dg"])
            yield P.group("pe", [mm(pD, qn, ba[:, 7, :], True, False), mm(pD, ba[:, 5, :], ba[:, 0, :], False, True)],
                          r=rq + ["Dg", "wneg", "AT"], w=["pD"])
            yield P.op("act", lambda e: e.copy(out=ba[:, 8, :], in_=pD), r=["pD"], w=["QeffT"])
            for c in ([0, 1] if d == 0 else [1, 0]):
                cs = slice(c * 64, (c + 1) * 64)
                kd = 6 if c == 0 else 10
                yield P.op("pe", mm(pE, ba[:, 5, :], ba[:, kd, :]), r=["wneg", "kdec"], w=["pE"])
                yield P.op("dve", lambda e: e.scalar_tensor_tensor(out=ba[:, 9, :], in0=identf[:], scalar=ds[:, 8 + c:9 + c], in1=pE, op0=ALU.mult, op1=ALU.add),
                           r=["pE", "egl", "identf"], w=["TrT"])
                yield P.group("pe", [mm(pF[0:64, :], ba[:, 0, cs], ba[:, 4, :], True, False), mm(pF[0:64, :], ba[:, 8, cs], Sb[:, h, :], False, True)],
                              r=["AT", "u", "QeffT", ("S", h)], w=["pF"])
                yield P.group("pe", [mm(pG, ba[:, kd, :], ba[:, 4, :], True, False), mm(pG, ba[:, 9, :], Sb[:, h, :], False, True)],
                              r=["kdec", "u", "TrT", ("S", h)], w=["pG"])
                yield P.op("act", lambda e: e.copy(out=osb[:, c, :], in_=pF[0:64, :]), r=["pF", "pG"], w=[("osb", c)])
                yield P.op("act", lambda e: e.copy(out=Sb[:, h, :], in_=pG), r=["pG"], w=[("S", h)])
                r0 = t * 128 + c * 64
                if d == 0:
                    yield P.dma("sp", lambda q: q.dma_start(out=D["o_f"][r0:r0 + 64, h * 128:(h + 1) * 128], in_=osb[:, c, :]), r=[("osb", c)], w=["o_f"])
                else:
                    yield P.dma("sp", lambda q: q.dma_start(out=ofl[:, c, :], in_=D["o_f"][r0:r0 + 64, h * 128:(h + 1) * 128]), r=["o_f"], w=[("ofl", c)])
                    yield P.dma("sp", lambda q: q.dma_start(out=zl[:, c, :], in_=D["dn_z"][r0:r0 + 64, h * 128:(h + 1) * 128]), r=["dn_z"], w=[("zl", c)])
                    yield P.op("dve", lambda e: e.tensor_tensor(out=ofl[:, c, :], in0=ofl[:, c, :], in1=osb[:, c, :], op=ALU.add), r=[("ofl", c), ("osb", c)], w=[("ofl", c)])
                    yield P.op("act", lambda e: e.activation(out=osb[:, c, :], in_=ofl[:, c, :], func=AF.Square, accum_out=ds[0:64, 12 + c:13 + c]), r=[("ofl", c)], w=[("osb", c), ("oss", c)])
                    yield P.op("act", lambda e: e.activation(out=ds[0:64, 14 + c:15 + c], in_=ds[0:64, 12 + c:13 + c], func=AF.Sqrt, bias=EPS, scale=1.0 / 128), r=[("oss", c)], w=[("ort", c)])
                    yield P.op("dve", lambda e: e.reciprocal(out=ds[0:64, 16 + c:17 + c], in_=ds[0:64, 14 + c:15 + c]), r=[("ort", c)], w=[("orr", c)])
                    yield P.op("dve", lambda e: e.scalar_tensor_tensor(out=ofl[:, c, :], in0=ofl[:, c, :], scalar=ds[0:64, 16 + c:17 + c], in1=onw[0:64, :], op0=ALU.mult, op1=ALU.mult),
                               r=[("ofl", c), ("orr", c), "onw"], w=[("ofl", c)])
                    yield P.op("dve", lambda e: e.tensor_tensor(out=oout[:, c, :], in0=ofl[:, c, :], in1=zl[:, c, :], op=ALU.mult), r=[("ofl", c), ("zl", c)], w=[("oout", c)])
                    yield P.dma("sp", lambda q: q.dma_start(out=D["oab"][r0:r0 + 64, h * 128:(h + 1) * 128], in_=oout[:, c, :]), r=[("oout", c)], w=["oab"])

        order = list(range(nt)) if d == 0 else list(range(nt - 1, -1, -1))
        for it, t in enumerate(order):
            b = it % 2
            rows = slice(t * 128, (t + 1) * 128)
            P.kp = None
            P.dma("sp", lambda q: q.dma_start(out=qk[b][:], in_=D["dn_qkv"][rows, :]), r=["dn_qkv"], w=[("qk", b)])
            P.dma("sp", lambda q: q.dma_start(out=scs[b][:], in_=D["dn_sc"][rows, :]), r=["dn_sc"], w=[("sc", b)])
            gens = [(h, unit(h, t, b)) for h in range(2)]
            while gens:
                for item in list(gens):
                    P.kp = item[0]
                    try:
                        next(item[1])
                    except StopIteration:
                        gens.remove(item)
            P.kp = None
        P.kp = None
import os as _os2
SWCUT = int(_os2.environ.get('SWCUT', '99'))
SWOPS = int(_os2.environ.get('SWOPS', '99'))
SWSKIP0 = int(_os2.environ.get('SWSKIP0', '0'))
SWALT = int(_os2.environ.get('SWALT', '0'))
SWNOCP = int(_os2.environ.get('SWNOCP', '0'))
SWMETA = int(_os2.environ.get('SWMETA', '1'))
def swa_pass(P, nc, D, C, nt=NT):
    NB = nt - 1
    scale = float(128 ** -0.5)
    with ExitStack() as ES:
        kt = ES.enter_context(nc.sbuf_tensor(_u("kt"), [128, 4, 512], BF16))
        KT = ES.enter_context(nc.sbuf_tensor(_u("KT"), [128, 4, 128], BF16))
        mq = ES.enter_context(nc.sbuf_tensor(_u("mq"), [16, 512], BF16))
        KTm = ES.enter_context(nc.sbuf_tensor(_u("KTm"), [128, 16], BF16))
        bias = ES.enter_context(nc.sbuf_tensor(_u("bias"), [128, 3, 2, 400], F32))
        mbias = ES.enter_context(nc.sbuf_tensor(_u("mbias"), [16, 144], F32))
        sink = ES.enter_context(nc.sbuf_tensor(_u("sink"), [128, 2], F32))
        Ss = ES.enter_context(nc.sbuf_tensor(_u("Ss"), [128, 400], F32))
        Pb = ES.enter_context(nc.sbuf_tensor(_u("Pb"), [128, 400], BF16))
        PT = ES.enter_context(nc.sbuf_tensor(_u("PT"), [128, 4, 128], BF16))
        QT = ES.enter_context(nc.sbuf_tensor(_u("QT"), [128, 128], BF16))
        ob = ES.enter_context(nc.sbuf_tensor(_u("ob"), [128, 128], BF16))
        ss = ES.enter_context(nc.sbuf_tensor(_u("ss"), [128, 16], F32))
        zt = ES.enter_context(nc.sbuf_tensor(_u("zt"), [128, 256], BF16))
        pS = ES.enter_context(nc.psum_tensor(_u("pS"), [128, 512], F32))
        pP = ES.enter_context(nc.psum_tensor(_u("pP"), [128, 512], BF16))
        pO = ES.enter_context(nc.psum_tensor(_u("pO"), [128, 512], F32))
        pK = ES.enter_context(nc.psum_tensor(_u("pK"), [128, 512], BF16))
        identb = C["identb"]
        P.dma("sp", lambda q: q.dma_start(out=bias[:], in_=D["swbias"]), w=["bias"])
        P.dma("sp", lambda q: q.dma_start(out=mbias[:], in_=D["swmbias"]), w=["mbias"])
        P.dma("sp", lambda q: q.dma_start(out=sink[:], in_=D["sinkbc"]), w=["sink"])
        P.dma("sp", lambda q: q.dma_start(out=mq[:], in_=D["sw_qkv"][112:128, :]), r=["sw_qkv"], w=["mq"])
        P.op("pool", lambda e: e.memset(zt[:], 0.0), w=["zt"])
        P.dma("sp", lambda q: q.dma_start(out=D["oab"][0:112, 256:512], in_=zt[0:112, :]), r=["zt"], w=["oab"])
        P.op("pe", lambda e: e.transpose(pK[:, 0:16], mq[:, 256:384], identb[0:16, 0:16]), r=["mq", "identb"], w=["pK"])
        P.op("act", lambda e: e.copy(out=KTm[:], in_=pK[:, 0:16]), r=["pK"], w=["KTm"])

        def load(t):
            s = t % 4
            P.dma("sp", lambda q: q.dma_start(out=kt[:, s, :], in_=D["sw_qkv"][t * 128:(t + 1) * 128, :]), r=["sw_qkv"], w=[("kt", s)])
            P.op("pe", lambda e: e.transpose(pK[:, 0:128], kt[:, s, 256:384], identb[:]), r=[("kt", s), "identb"], w=["pK"])
            P.op("act", lambda e: e.copy(out=KT[:, s, :], in_=pK[:, 0:128]), r=["pK"], w=[("KT", s)])

        def attend(np_, qsrc, qkey, keysets, bias_ap, sink_ap, out_ap):
            ntot = sum(k[2] for k in keysets)
            deps = [qkey] + [dd for k in keysets for dd in k[3]]
            P.op("pe", lambda e: e.transpose(pP[:, 0:np_], qsrc, identb[0:np_, 0:np_]), r=[qkey, "identb"], w=["pP"])
            P.op("act", lambda e: e.copy(out=QT[:, 0:np_], in_=pP[:, 0:np_]), r=["pP"], w=["QT"])
            if SWCUT < 2:
                return
            fns = []
            c0 = 0
            for (ka, va, nk, _) in keysets:
                fns.append(mm(pS[0:np_, c0:c0 + nk], QT[:, 0:np_], ka))
                c0 += nk
            P.group("pe", fns, r=["QT"] + deps, w=["pS"])
            if SWCUT < 3:
                return
            if SWOPS < 1:
                return
            P.op("dve", lambda e: e.scalar_tensor_tensor(out=Ss[0:np_, 0:ntot], in0=pS[0:np_, 0:ntot], scalar=scale, in1=bias_ap, op0=ALU.mult, op1=ALU.add),
                 r=["pS", "bias", "mbias"], w=["Ss"])
            if SWOPS < 2:
                return
            P.op("dve", lambda e: e.reduce_max(out=ss[0:np_, 0:1], in_=Ss[0:np_, 0:ntot], axis=AX.X), r=["Ss"], w=["mx"])
            if SWOPS < 3:
                return
            P.op("dve", lambda e: e.tensor_tensor(out=ss[0:np_, 1:2], in0=ss[0:np_, 0:1], in1=sink_ap, op=ALU.max), r=["mx", "sink"], w=["m"])
            if SWOPS < 4:
                return
            P.op("dve", lambda e: e.tensor_scalar(out=ss[0:np_, 2:3], in0=ss[0:np_, 1:2], scalar1=-1.0, scalar2=None, op0=ALU.mult), r=["m"], w=["negm"])
            if SWOPS < 5:
                return
            P.op("act", lambda e: e.activation(out=Pb[0:np_, 0:ntot], in_=Ss[0:np_, 0:ntot], func=AF.Exp, bias=ss[0:np_, 2:3], accum_out=ss[0:np_, 3:4]),
                 r=["Ss", "negm"], w=["Pb", "rs"])
            if SWOPS < 6:
                return
            P.op("act", lambda e: e.activation(out=ss[0:np_, 4:5], in_=ss[0:np_, 2:3], func=AF.Exp, bias=sink_ap), r=["negm", "sink"], w=["es"])
            if SWOPS < 7:
                return
            P.op("dve", lambda e: e.tensor_tensor(out=ss[0:np_, 5:6], in0=ss[0:np_, 3:4], in1=ss[0:np_, 4:5], op=ALU.add), r=["rs", "es"], w=["den"])
            if SWOPS < 8:
                return
            P.op("dve", lambda e: e.reciprocal(out=ss[0:np_, 6:7], in_=ss[0:np_, 5:6]), r=["den"], w=["rden"])
            if SWCUT < 4:
                return
            fns = []
            c0 = 0
            for j, (ka, va, nk, _) in enumerate(keysets):
                if not (SWSKIP0 and nk == 16):
                    fns.append(lambda e, j=j, c0=c0, nk=nk: e.transpose(pP[0:nk, j * 128:j * 128 + np_], (kt[0:np_, 0, c0:c0 + nk] if SWALT else Pb[0:np_, c0:c0 + nk]), identb[0:np_, 0:np_]))
                c0 += nk
            P.group("pe", fns, r=["Pb", "identb", "QT"], w=["pP"])
            for j, (ka, va, nk, _) in enumerate(keysets):
                if SWNOCP or (SWSKIP0 and nk == 16):
                    continue
                P.op("act", (lambda e, j=j, nk=nk: e.copy(out=PT[0:nk, j, 0:np_], in_=pP[0:nk, j * 128:j * 128 + np_])), r=["pP"], w=["PT"])
            if SWCUT < 5:
                return
            fns = []
            for j, (ka, va, nk, _) in enumerate(keysets):
                fns.append(mm(pO[0:np_, 0:128], PT[0:nk, j, 0:np_], va, start=(j == 0), stop=(j == len(keysets) - 1)))
            P.group("pe", fns, r=["PT"] + deps, w=["pO"])
            P.op("act", lambda e: e.activation(out=ob[0:np_, :], in_=pO[0:np_, 0:128], func=AF.Copy, scale=ss[0:np_, 6:7]), r=["pO", "rden"], w=["ob"])
            P.dma("sp", lambda q: q.dma_start(out=out_ap, in_=ob[0:np_, :]), r=["ob"], w=["oab"])

        load(0)
        load(1)
        meta_ks = (KTm[:, 0:16], mq[0:16, 384:512], 16, ["KTm", "mq"])
        for h in (range(2) if SWMETA else []):
            attend(16, mq[0:16, h * 128:(h + 1) * 128], "mq",
                   [(KT[:, 1, :], kt[:, 1, 384:512], 128, [("KT", 1), ("kt", 1)]), meta_ks],
                   mbias[:, :], sink[0:16, h:h + 1], D["oab"][112:128, 256 + h * 128:256 + (h + 1) * 128])
        for n in range(NB):
            t = n + 1
            if t + 1 < nt:
                load(t + 1)
            var = 0 if n == 0 else (2 if n == NB - 1 else 1)
            tn = t + 1 if t + 1 < nt else t
            sl = [(t - 1) % 4, t % 4, tn % 4]
            for h in range(2):
                ks = [(KT[:, s, :], kt[:, s, 384:512], 128, [("KT", s), ("kt", s)]) for s in sl] + [meta_ks]
                attend(128, kt[:, t % 4, h * 128:(h + 1) * 128], ("kt", t % 4), ks, bias[:, var, h, :], sink[:, h:h + 1],
                       D["oab"][t * 128:(t + 1) * 128, 256 + h * 128:256 + (h + 1) * 128])
def allgather(P, src, dst, rkeys, wkeys):
    P.dma("pool", lambda q: q.collective_compute("AllGather", ALU.bypass, replica_groups=[list(range(8))],
                                                 ins=[src.opt()], outs=[dst.opt()]), r=rkeys, w=wkeys, inc=1)


def transpose_into(P, src_fn, dstT, nch, tp, identb, rkeys, wkey, evac=("act", "dve")):
    for g in range((nch + 3) // 4):
        tb = g % 2
        n = min(4, nch - 4 * g)
        P.group("pe", [(lambda e, j=j: e.transpose(tp[tb][:, j * 128:(j + 1) * 128], src_fn(4 * g + j), identb[:])) for j in range(n)],
                r=rkeys + ["identb"], w=[("tp", tb)])
        src = tp[tb][:, 0:n * 128].rearrange("p (a b) -> p a b", b=128)
        if evac[g % 2] == "act":
            P.op("act", lambda e: e.copy(out=dstT[:, 4 * g:4 * g + n, :], in_=src), r=[("tp", tb)], w=[wkey])
        else:
            P.op("dve", lambda e: e.tensor_copy(out=dstT[:, 4 * g:4 * g + n, :], in_=src), r=[("tp", tb)], w=[wkey])


def phase_B1(P, nc, D, C, nt=NT):
    with ExitStack() as ES:
        Wga = ES.enter_context(nc.sbuf_tensor(_u("Wga"), [128, 32, 512], BF16))
        Wgb = ES.enter_context(nc.sbuf_tensor(_u("Wgb"), [128, 32, 512], BF16))
        Wa = ES.enter_context(nc.sbuf_tensor(_u("Wa"), [128, 16, 512], BF16))
        Wb = ES.enter_context(nc.sbuf_tensor(_u("Wb"), [128, 16, 512], BF16))
        xt0 = ES.enter_context(nc.sbuf_tensor(_u("xt0"), [128, 512], F32))
        xt1 = ES.enter_context(nc.sbuf_tensor(_u("xt1"), [128, 512], F32))
        xTa = ES.enter_context(nc.sbuf_tensor(_u("xTa"), [128, 32, 128], BF16))
        xTb = ES.enter_context(nc.sbuf_tensor(_u("xTb"), [128, 32, 128], BF16))
        ob0 = ES.enter_context(nc.sbuf_tensor(_u("ob0"), [128, 8, 512], BF16))
        ob1 = ES.enter_context(nc.sbuf_tensor(_u("ob1"), [128, 8, 512], BF16))
        oT = ES.enter_context(nc.sbuf_tensor(_u("oT"), [128, 32, 128], BF16))
        sg = ES.enter_context(nc.sbuf_tensor(_u("sg"), [128, 2, 512], F32))
        mxb = ES.enter_context(nc.sbuf_tensor(_u("mx"), [128, 512], BF16))
        sm = ES.enter_context(nc.sbuf_tensor(_u("sm"), [128, 8], F32))
        w1c = ES.enter_context(nc.sbuf_tensor(_u("w1c"), [128, 32], F32))
        tp0 = ES.enter_context(nc.psum_tensor(_u("tp0"), [128, 512], BF16))
        tp1 = ES.enter_context(nc.psum_tensor(_u("tp1"), [128, 512], BF16))
        p0 = ES.enter_context(nc.psum_tensor(_u("p0"), [128, 512], F32))
        p1 = ES.enter_context(nc.psum_tensor(_u("p1"), [128, 512], F32))
        p2 = ES.enter_context(nc.psum_tensor(_u("p2"), [128, 512], F32))
        p3 = ES.enter_context(nc.psum_tensor(_u("p3"), [128, 512], F32))
        xt = [xt0, xt1]
        ob = [ob0, ob1]
        tp = [tp0, tp1]
        identb = C["identb"]
        P.dma("sp", lambda q: q.dma_start(out=w1c[:], in_=D["w1c"]), w=["w1c"])
        for wi, (Wt, nm) in enumerate([(Wga, "wga"), (Wgb, "wgb")]):
            for kc in range(32):
                b = kc % 2
                P.dma("sp", lambda q: q.dma_start(out=xt[b][:, 0:512], in_=D[nm][kc * 128:(kc + 1) * 128, :]), w=[("xt", b)])
                P.op("act", lambda e: e.activation(out=Wt[:, kc, :], in_=xt[b][:, 0:512], func=AF.Copy, scale=w1c[:, kc:kc + 1]),
                     r=[("xt", b), "w1c"], w=["W" + nm])
        P.dma("pool", lambda q: q.dma_start(out=Wa[:], in_=D["wa"].rearrange("(kc p) n -> p kc n", p=128)), w=["Wwa"])
        P.dma("pool", lambda q: q.dma_start(out=Wb[:], in_=D["wb"].rearrange("(kc p) n -> p kc n", p=128)), w=["Wwb"])
        for t in range(nt):
            b = t % 2
            rows = slice(t * 128, (t + 1) * 128)
            xT = [xTa, xTb][b]
            P.dma("pool", lambda q: q.dma_start(out=xT[:].rearrange("p a b -> p (a b)"), in_=D["xT_s"][rows, :]), r=["xT_s"], w=[("xT", b)])
            for r in range(8):
                P.dma("sp", lambda q: q.dma_start(out=ob[b][:, r, :], in_=D["oab_all"][r * NR + t * 128:r * NR + (t + 1) * 128, :]), r=["oab_all"], w=[("ob", b)])
            transpose_into(P, lambda i: ob[b][:, (i % 16) // 2, (i // 16) * 256 + (i % 2) * 128:(i // 16) * 256 + (i % 2) * 128 + 128], oT, 32, tp, identb, [("ob", b)], "oT")
            P.group("pe", [mm(p0[:], xT[:, kc, :], Wga[:, kc, :], kc == 0, kc == 31) for kc in range(32)], r=[("xT", b), "Wwga"], w=["p0"])
            P.group("pe", [mm(p1[:], xT[:, kc, :], Wgb[:, kc, :], kc == 0, kc == 31) for kc in range(32)], r=[("xT", b), "Wwgb"], w=["p1"])
            P.group("pe", [mm(p2[:], oT[:, kc, :], Wa[:, kc, :], kc == 0, kc == 15) for kc in range(16)], r=["oT", "Wwa"], w=["p2"])
            P.group("pe", [mm(p3[:], oT[:, 16 + kc, :], Wb[:, kc, :], kc == 0, kc == 15) for kc in range(16)], r=["oT", "Wwb"], w=["p3"])
            P.op("act", lambda e: e.activation(out=sg[:, 0, :], in_=p0[:], func=AF.Sigmoid), r=["p0"], w=[("sg", 0)])
            P.op("act", lambda e: e.activation(out=sg[:, 1, :], in_=p1[:], func=AF.Sigmoid), r=["p1"], w=[("sg", 1)])
            P.op("dve", lambda e: e.tensor_tensor(out=sg[:, 0, :], in0=sg[:, 0, :], in1=p2[:], op=ALU.mult), r=[("sg", 0), "p2"], w=[("sg", 0)])
            P.op("dve", lambda e: e.tensor_tensor(out=sg[:, 1, :], in0=sg[:, 1, :], in1=p3[:], op=ALU.mult), r=[("sg", 1), "p3"], w=[("sg", 1)])
            P.op("pool", lambda e: e.tensor_tensor(out=mxb[:], in0=sg[:, 0, :], in1=sg[:, 1, :], op=ALU.add), r=[("sg", 0), ("sg", 1)], w=["mxb"])
            P.dma("sp", lambda q: q.dma_start(out=D["mixed"][rows, :], in_=mxb[:]), r=["mxb"], w=["mixed"])


def phase_B2(P, nc, D, C, nt=NT):
    with ExitStack() as ES:
        Wo = ES.enter_context(nc.sbuf_tensor(_u("Wo"), [128, 32, 512], BF16))
        mb0 = ES.enter_context(nc.sbuf_tensor(_u("mb0"), [128, 8, 512], BF16))
        mb1 = ES.enter_context(nc.sbuf_tensor(_u("mb1"), [128, 8, 512], BF16))
        mT = ES.enter_context(nc.sbuf_tensor(_u("mT"), [128, 32, 128], BF16))
        xc0 = ES.enter_context(nc.sbuf_tensor(_u("xc0"), [128, 512], F32))
        xc1 = ES.enter_context(nc.sbuf_tensor(_u("xc1"), [128, 512], F32))
        h2o = ES.enter_context(nc.sbuf_tensor(_u("h2o"), [128, 512], F32))
        tp0 = ES.enter_context(nc.psum_tensor(_u("tp0"), [128, 512], BF16))
        tp1 = ES.enter_context(nc.psum_tensor(_u("tp1"), [128, 512], BF16))
        p0 = ES.enter_context(nc.psum_tensor(_u("p0"), [128, 512], F32))
        mb = [mb0, mb1]
        xc = [xc0, xc1]
        tp = [tp0, tp1]
        identb = C["identb"]
        P.dma("pool", lambda q: q.dma_start(out=Wo[:], in_=D["wo"].rearrange("(kc p) n -> p kc n", p=128)), w=["Wo"])
        for t in range(nt):
            b = t % 2
            rows = slice(t * 128, (t + 1) * 128)
            for r in range(8):
                P.dma("sp", lambda q: q.dma_start(out=mb[b][:, r, :], in_=D["mixed_all"][r * NR + t * 128:r * NR + (t + 1) * 128, :]), r=["mixed_all"], w=[("mb", b)])
            P.dma("sp", lambda q: q.dma_start(out=xc[b][:], in_=D["xcol"][rows, :]), w=[("xc", b)])
            transpose_into(P, lambda i: mb[b][:, i // 4, (i % 4) * 128:(i % 4) * 128 + 128], mT, 32, tp, identb, [("mb", b)], "mT")
            P.group("pe", [mm(p0[:], mT[:, kc, :], Wo[:, kc, :], kc == 0, kc == 31) for kc in range(32)], r=["mT", "Wo"], w=["p0"])
            P.op("dve", lambda e: e.tensor_tensor(out=h2o[:], in0=xc[b][:], in1=p0[:], op=ALU.add), r=[("xc", b), "p0"], w=["h2o"])
            P.dma("sp", lambda q: q.dma_start(out=D["h2c"][rows, :], in_=h2o[:]), r=["h2o"], w=["h2c"])


def phase_B3(P, nc, D, C, affs, nt=NT):
    with ExitStack() as ES:
        h0 = ES.enter_context(nc.sbuf_tensor(_u("h0"), [128, 4096], F32))
        h1 = ES.enter_context(nc.sbuf_tensor(_u("h1"), [128, 4096], F32))
        hnf = ES.enter_context(nc.sbuf_tensor(_u("hnf"), [128, 4096], F32))
        w2 = ES.enter_context(nc.sbuf_tensor(_u("w2"), [128, 4096], F32))
        hnb = ES.enter_context(nc.sbuf_tensor(_u("hnb"), [128, 4096], BF16))
        hT = ES.enter_context(nc.sbuf_tensor(_u("hT"), [128, 32, 128], F32))
        wr = ES.enter_context(nc.sbuf_tensor(_u("wr"), [128, 32, 16], F32))
        sm = ES.enter_context(nc.sbuf_tensor(_u("sm"), [128, 32], F32))
        tq0 = ES.enter_context(nc.psum_tensor(_u("tq0"), [128, 512], F32))
        tq1 = ES.enter_context(nc.psum_tensor(_u("tq1"), [128, 512], F32))
        pl = ES.enter_context(nc.psum_tensor(_u("pl"), [128, 512], F32))
        hh = [h0, h1]
        tq = [tq0, tq1]
        identf = C["identf"]
        P.dma("sp", lambda q: q.dma_start(out=w2[:], in_=D["w2bc"]), w=["w2"])
        P.dma("sp", lambda q: q.dma_start(out=wr[:], in_=D["wr"].rearrange("(kc p) e -> p kc e", p=128)), w=["wr"])
        for t in range(nt):
            b = t % 2
            rows = slice(t * 128, (t + 1) * 128)
            for r in range(8):
                P.dma("sp", lambda q: q.dma_start(out=hh[b][:, r * 512:(r + 1) * 512], in_=D["h2_all"][r * NR + t * 128:r * NR + (t + 1) * 128, :]), r=["h2_all"], w=[("hh", b)])
            P.op("act", lambda e: e.activation(out=hnb[:], in_=hh[b][:], func=AF.Square, accum_out=sm[:, 0:1]), r=[("hh", b)], w=["hnb", "ss"])
            P.op("act", lambda e: e.activation(out=sm[:, 1:2], in_=sm[:, 0:1], func=AF.Sqrt, bias=EPS, scale=1.0 / 4096), r=["ss"], w=["rt"])
            P.op("dve", lambda e: e.reciprocal(out=sm[:, 2:3], in_=sm[:, 1:2]), r=["rt"], w=["rstd"])
            P.op("dve", lambda e: e.scalar_tensor_tensor(out=hnf[:], in0=hh[b][:], scalar=sm[:, 2:3], in1=w2[:], op0=ALU.mult, op1=ALU.mult), r=[("hh", b), "rstd", "w2"], w=["hnf"])
            P.op("act", lambda e: e.copy(out=hnb[:], in_=hnf[:]), r=["hnf"], w=["hnb"])
            P.dma("sp", lambda q: q.dma_start(out=D["hn2"][rows, :], in_=hnb[:]), r=["hnb"], w=["hn2"])
            for g in range(8):
                tb = g % 2
                P.group("pe", [(lambda e, j=j: e.transpose(tq[tb][:, j * 128:(j + 1) * 128], hnf[:, (4 * g + j) * 128:(4 * g + j + 1) * 128], identf[:])) for j in range(4)],
                        r=["hnf", "identf"], w=[("tq", tb)])
                src = tq[tb][:].rearrange("p (a b) -> p a b", b=128)
                if g % 2 == 0:
                    P.op("act", lambda e: e.copy(out=hT[:, 4 * g:4 * g + 4, :], in_=src), r=[("tq", tb)], w=["hT"])
                else:
                    P.op("dve", lambda e: e.tensor_copy(out=hT[:, 4 * g:4 * g + 4, :], in_=src), r=[("tq", tb)], w=["hT"])
            P.group("pe", [mm(pl[:, 0:16], hT[:, kc, :], wr[:, kc, :], kc == 0, kc == 31) for kc in range(32)], r=["hT", "wr"], w=["pl"])
            P.op("dve", lambda e: e.reduce_max(out=sm[:, 3:4], in_=pl[:, 0:16], axis=AX.X), r=["pl"], w=["mx"])
            P.op("dve", lambda e: e.tensor_scalar(out=sm[:, 4:5], in0=sm[:, 3:4], scalar1=-1.0, scalar2=None, op0=ALU.mult), r=["mx"], w=["negm"])
            P.op("act", lambda e: e.activation(out=sm[:, 8:24], in_=pl[:, 0:16], func=AF.Exp, bias=sm[:, 4:5], accum_out=sm[:, 5:6]), r=["pl", "negm"], w=["ex", "es"])
            P.op("dve", lambda e: e.reciprocal(out=sm[:, 6:7], in_=sm[:, 5:6]), r=["es"], w=["rs"])
            P.op("dve", lambda e: e.tensor_scalar(out=affs[:, t, :], in0=sm[:, 8:24], scalar1=sm[:, 6:7], scalar2=None, op0=ALU.mult), r=["ex", "rs"], w=["affs"])


CAP = 1026
BIG = float(1 << 20)


def phase_topk(P, nc, D, C, affs, sloti, nt=NT):
    n3 = nt * 16
    with ExitStack() as ES:
        cmp_ = ES.enter_context(nc.sbuf_tensor(_u("cmp"), [128, nt, 16], F32))
        val = ES.enter_context(nc.sbuf_tensor(_u("val"), [128, nt, 16], F32))
        pos = ES.enter_context(nc.sbuf_tensor(_u("pos"), [128, nt, 16], F32))
        tt = ES.enter_context(nc.sbuf_tensor(_u("tt"), [128, nt, 16], F32))
        cs = ES.enter_context(nc.sbuf_tensor(_u("cs"), [128, nt, 16], F32))
        one = ES.enter_context(nc.sbuf_tensor(_u("one"), [128, nt], F32))
        lst = ES.enter_context(nc.sbuf_tensor(_u("lst"), [128, 128], F32))
        bb = ES.enter_context(nc.sbuf_tensor(_u("b"), [128, 8, 16], F32))
        pc = ES.enter_context(nc.psum_tensor(_u("pc"), [128, 512], F32))
        pw0 = ES.enter_context(nc.psum_tensor(_u("pw0"), [128, 512], F32))
        pw1 = ES.enter_context(nc.psum_tensor(_u("pw1"), [128, 512], F32))
        pw2 = ES.enter_context(nc.psum_tensor(_u("pw2"), [128, 512], F32))
        onesf = C["onesf"]
        P.dma("sp", lambda q: q.dma_start(out=val[:], in_=D["validbc"]), w=["val"])
        P.dma("sp", lambda q: q.dma_start(out=lst[:], in_=D["lstrict"]), w=["lst"])
        P.op("dve", lambda e: e.tensor_tensor(out=affs[:], in0=affs[:], in1=val[:], op=ALU.mult), r=["affs", "val"], w=["affs"])
        P.op("pool", lambda e: e.memset(bb[:, 0, :], 0.0), w=["lo"])
        P.op("pool", lambda e: e.memset(bb[:, 1, :], 1.0), w=["hi"])
        P.op("pool", lambda e: e.memset(one[:], 1.0), w=["one"])
        affs_et = affs[:].rearrange("p t e -> p e t")

        def bc(ap):
            return ap.unsqueeze(1).broadcast_to([128, nt, 16])

        for it in range(34):
            P.op("dve", lambda e: e.tensor_tensor(out=bb[:, 2, :], in0=bb[:, 0, :], in1=bb[:, 1, :], op=ALU.add), r=["lo", "hi"], w=["mid"])
            P.op("dve", lambda e: e.tensor_scalar(out=bb[:, 2, :], in0=bb[:, 2, :], scalar1=0.5, scalar2=None, op0=ALU.mult), r=["mid"], w=["mid"])
            P.op("dve", lambda e: e.tensor_tensor(out=cmp_[:], in0=affs[:], in1=bc(bb[:, 2, :]), op=ALU.is_ge), r=["affs", "mid"], w=["cmp"])
            P.op("dve", lambda e: e.tensor_reduce(out=bb[:, 3, :], in_=cmp_[:].rearrange("p t e -> p e t"), axis=AX.X, op=ALU.add), r=["cmp"], w=["cnt"])
            P.op("pe", mm(pc[:, 0:16], onesf[:], bb[:, 3, :]), r=["cnt", "onesf"], w=["pc"])
            P.op("dve", lambda e: e.tensor_single_scalar(out=bb[:, 4, :], in_=pc[:, 0:16], scalar=CAP - 0.5, op=ALU.is_ge), r=["pc"], w=["ge"])
            P.op("dve", lambda e: e.tensor_tensor(out=bb[:, 5, :], in0=bb[:, 2, :], in1=bb[:, 0, :], op=ALU.subtract), r=["mid", "lo"], w=["d1"])
            P.op("dve", lambda e: e.tensor_tensor(out=bb[:, 5, :], in0=bb[:, 5, :], in1=bb[:, 4, :], op=ALU.mult), r=["d1", "ge"], w=["d1"])
            P.op("dve", lambda e: e.tensor_tensor(out=bb[:, 6, :], in0=bb[:, 1, :], in1=bb[:, 2, :], op=ALU.subtract), r=["mid", "hi"], w=["d2"])
            P.op("dve", lambda e: e.tensor_tensor(out=bb[:, 6, :], in0=bb[:, 6, :], in1=bb[:, 4, :], op=ALU.mult), r=["d2", "ge"], w=["d2"])
            P.op("dve", lambda e: e.tensor_tensor(out=bb[:, 0, :], in0=bb[:, 0, :], in1=bb[:, 5, :], op=ALU.add), r=["lo", "d1"], w=["lo"])
            P.op("dve", lambda e: e.tensor_tensor(out=bb[:, 1, :], in0=bb[:, 2, :], in1=bb[:, 6, :], op=ALU.add), r=["mid", "d2"], w=["hi"])
        P.op("dve", lambda e: e.tensor_tensor(out=cmp_[:], in0=affs[:], in1=bc(bb[:, 0, :]), op=ALU.is_ge), r=["affs", "lo"], w=["cmp"])
        cf = cmp_[:].rearrange("p t e -> p (t e)")
        pws = [pw0, pw1, pw2]
        for i in range(3):
            c0, c1 = i * 512, min(n3, (i + 1) * 512)
            P.op("pe", mm(pws[i][:, 0:c1 - c0], lst[:], cf[:, c0:c1]), r=["cmp", "lst"], w=[("pw", i)])
            P.op("act", lambda e: e.copy(out=pos[:].rearrange("p t e -> p (t e)")[:, c0:c1], in_=pws[i][:, 0:c1 - c0]), r=[("pw", i)], w=["pos"])
        for i in range(3):
            c0, c1 = i * 512, min(n3, (i + 1) * 512)
            P.op("pe", mm(pws[i][:, 0:c1 - c0], onesf[:], cf[:, c0:c1]), r=["cmp", "onesf", "pos"], w=[("pw", i)])
            P.op("act", lambda e: e.copy(out=tt[:].rearrange("p t e -> p (t e)")[:, c0:c1], in_=pws[i][:, 0:c1 - c0]), r=[("pw", i)], w=["tt"])
        for ex in range(16):
            P.op("dve", lambda e: e.tensor_tensor_scan(out=cs[:, :, ex], data0=one[:], data1=tt[:, :, ex], initial=0.0, op0=ALU.mult, op1=ALU.add), r=["tt", "one"], w=["cs"])
        P.op("dve", lambda e: e.tensor_tensor(out=cs[:], in0=cs[:], in1=tt[:], op=ALU.subtract), r=["cs", "tt"], w=["cs"])
        P.op("dve", lambda e: e.tensor_tensor(out=pos[:], in0=pos[:], in1=cs[:], op=ALU.add), r=["pos", "cs"], w=["pos"])
        P.op("dve", lambda e: e.tensor_scalar(out=cmp_[:], in0=cmp_[:], scalar1=-BIG, scalar2=BIG, op0=ALU.mult, op1=ALU.add), r=["cmp"], w=["cmp"])
        P.op("dve", lambda e: e.tensor_tensor(out=pos[:], in0=pos[:], in1=cmp_[:], op=ALU.add), r=["pos", "cmp"], w=["pos"])
        P.op("dve", lambda e: e.tensor_scalar(out=cmp_[:], in0=pos[:], scalar1=CAP - 0.5, scalar2=BIG, op0=ALU.is_ge, op1=ALU.mult), r=["pos", "cmp"], w=["cmp"])
        P.op("dve", lambda e: e.tensor_tensor(out=pos[:], in0=pos[:], in1=cmp_[:], op=ALU.add), r=["pos", "cmp"], w=["pos"])
        P.op("dve", lambda e: e.tensor_copy(out=sloti[:], in_=pos[:]), r=["pos"], w=["sloti"])
        P.dma("sp", lambda q: q.dma_start(out=D["slot_tab"].rearrange("(t p) e -> p t e", p=128), in_=pos[:]), r=["pos"], w=["slot_tab"])
        P.dma("sp", lambda q: q.dma_start(out=D["aff_tab"].rearrange("(t p) e -> p t e", p=128), in_=affs[:]), r=["affs"], w=["aff_tab"])
_BR = {}
def BREG(nc, v):
    k = (id(nc), v)
    if k not in _BR:
        _BR[k] = nc.gpsimd.to_reg(v)
    return _BR[k]

NS = 1152


def phase_C(P, nc, D, C, sloti, nt=NT):
    identb = C["identb"]
    with ExitStack() as ES:
        hb0 = ES.enter_context(nc.sbuf_tensor(_u("hb0"), [128, 4096], BF16))
        hb1 = ES.enter_context(nc.sbuf_tensor(_u("hb1"), [128, 4096], BF16))
        hb = [hb0, hb1]
        for t in range(nt):
            b = t % 2
            P.dma("sp", lambda q: q.dma_start(out=hb[b][:], in_=D["hn2"][t * 128:(t + 1) * 128, :]), r=["hn2"], w=[("hb", b)])
            for j in range(2):
                P.dma("pool", lambda q: q.indirect_dma_start(out=D["Xe%d" % j], out_offset=bass.IndirectOffsetOnAxis(ap=sloti[:, t, j:j + 1], axis=0),
                                                             in_=hb[b][:], in_offset=None, bounds_check=BREG(nc, CAP - 1), oob_is_err=False),
                      r=[("hb", b), "sloti"], w=["Xe%d" % j])
    P.barrier()
    with ExitStack() as ES:
        XeT = ES.enter_context(nc.sbuf_tensor(_u("XeT"), [128, 32, NS], BF16))
        hT = ES.enter_context(nc.sbuf_tensor(_u("hT"), [128, 16, NS], BF16))
        xl0 = ES.enter_context(nc.sbuf_tensor(_u("xl0"), [128, 4096], BF16))
        xl1 = ES.enter_context(nc.sbuf_tensor(_u("xl1"), [128, 4096], BF16))
        wg0 = ES.enter_context(nc.sbuf_tensor(_u("wg0"), [128, 32, 128], BF16))
        wg1 = ES.enter_context(nc.sbuf_tensor(_u("wg1"), [128, 32, 128], BF16))
        wu0 = ES.enter_context(nc.sbuf_tensor(_u("wu0"), [128, 32, 128], BF16))
        wu1 = ES.enter_context(nc.sbuf_tensor(_u("wu1"), [128, 32, 128], BF16))
        wd0 = ES.enter_context(nc.sbuf_tensor(_u("wd0"), [128, 16, 512], BF16))
        wd1 = ES.enter_context(nc.sbuf_tensor(_u("wd1"), [128, 16, 512], BF16))
        sil = ES.enter_context(nc.sbuf_tensor(_u("sil"), [128, 384], F32))
        yo0 = ES.enter_context(nc.sbuf_tensor(_u("yo0"), [128, 512], BF16))
        yo1 = ES.enter_context(nc.sbuf_tensor(_u("yo1"), [128, 512], BF16))
        tp0 = ES.enter_context(nc.psum_tensor(_u("tp0"), [128, 512], BF16))
        tp1 = ES.enter_context(nc.psum_tensor(_u("tp1"), [128, 512], BF16))
        pg0 = ES.enter_context(nc.psum_tensor(_u("pg0"), [128, 512], F32))
        pg1 = ES.enter_context(nc.psum_tensor(_u("pg1"), [128, 512], F32))
        pu0 = ES.enter_context(nc.psum_tensor(_u("pu0"), [128, 512], F32))
        pu1 = ES.enter_context(nc.psum_tensor(_u("pu1"), [128, 512], F32))
        py0 = ES.enter_context(nc.psum_tensor(_u("py0"), [128, 512], F32))
        py1 = ES.enter_context(nc.psum_tensor(_u("py1"), [128, 512], F32))
        xl = [xl0, xl1]
        tp = [tp0, tp1]
        wgs, wus, wds = [wg0, wg1], [wu0, wu1], [wd0, wd1]
        pgs, pus, pys, yos = [pg0, pg1], [pu0, pu1], [py0, py1], [yo0, yo1]
        cnt = 0
        for j in range(2):
            for st in range(9):
                b = st % 2
                P.dma("sp", lambda q: q.dma_start(out=xl[b][:], in_=D["Xe%d" % j][st * 128:(st + 1) * 128, :]), r=["Xe%d" % j], w=[("xl", b)])
                for g in range(8):
                    tb = g % 2
                    P.group("pe", [(lambda e, jj=jj: e.transpose(tp[tb][:, jj * 128:(jj + 1) * 128], xl[b][:, (4 * g + jj) * 128:(4 * g + jj + 1) * 128], identb[:])) for jj in range(4)],
                            r=[("xl", b), "identb"], w=[("tp", tb)])
                    src = tp[tb][:].rearrange("p (a b) -> p a b", b=128)
                    if g % 2 == 0:
                        P.op("act", lambda e: e.copy(out=XeT[:, 4 * g:4 * g + 4, st * 128:(st + 1) * 128], in_=src), r=[("tp", tb)], w=["XeT"])
                    else:
                        P.op("dve", lambda e: e.tensor_copy(out=XeT[:, 4 * g:4 * g + 4, st * 128:(st + 1) * 128], in_=src), r=[("tp", tb)], w=["XeT"])
            for fc in range(16):
                wb = fc % 2
                P.dma("pool", lambda q: q.dma_start(out=wgs[wb][:], in_=D["wg"][j].rearrange("(kc p) f -> p kc f", p=128)[:, :, fc * 128:(fc + 1) * 128]), w=[("wg", wb)])
                P.dma("pool", lambda q: q.dma_start(out=wus[wb][:], in_=D["wu"][j].rearrange("(kc p) f -> p kc f", p=128)[:, :, fc * 128:(fc + 1) * 128]), w=[("wu", wb)])
                for sgi in range(3):
                    pb = cnt % 2
                    cnt += 1
                    ssl = slice(sgi * 384, (sgi + 1) * 384)
                    P.group("pe", [mm(pgs[pb][:, 0:384], wgs[wb][:, kc, :], XeT[:, kc, ssl], kc == 0, kc == 31) for kc in range(32)], r=[("wg", wb), "XeT"], w=[("pg", pb)])
                    P.group("pe", [mm(pus[pb][:, 0:384], wus[wb][:, kc, :], XeT[:, kc, ssl], kc == 0, kc == 31) for kc in range(32)], r=[("wu", wb), "XeT"], w=[("pu", pb)])
                    P.op("act", lambda e: e.activation(out=sil[:], in_=pgs[pb][:, 0:384], func=AF.Silu), r=[("pg", pb)], w=["sil"])
                    P.op("dve", lambda e: e.tensor_tensor(out=hT[:, fc, ssl], in0=sil[:], in1=pus[pb][:, 0:384], op=ALU.mult), r=["sil", ("pu", pb)], w=["hT"])
            for nb in range(8):
                wb = nb % 2
                P.dma("pool", lambda q: q.dma_start(out=wds[wb][:], in_=D["wd"][j].rearrange("(fc p) n -> p fc n", p=128)[:, :, nb * 512:(nb + 1) * 512]), w=[("wd", wb)])
                for st in range(9):
                    pb = cnt % 2
                    cnt += 1
                    P.group("pe", [mm(pys[pb][:], hT[:, fc, st * 128:(st + 1) * 128], wds[wb][:, fc, :], fc == 0, fc == 15) for fc in range(16)], r=["hT", ("wd", wb)], w=[("py", pb)])
                    if pb == 0:
                        P.op("act", lambda e: e.copy(out=yos[pb][:], in_=pys[pb][:]), r=[("py", pb)], w=[("yo", pb)])
                    else:
                        P.op("dve", lambda e: e.tensor_copy(out=yos[pb][:], in_=pys[pb][:]), r=[("py", pb)], w=[("yo", pb)])
                    P.dma("sp", lambda q: q.dma_start(out=D["ye"][j * NS + st * 128:j * NS + (st + 1) * 128, nb * 512:(nb + 1) * 512], in_=yos[pb][:]), r=[("yo", pb)], w=["ye"])


def phase_D(P, nc, D, C):
    with ExitStack() as ES:
        acc = ES.enter_context(nc.sbuf_tensor(_u("acc"), [128, 4096], F32))
        gb0 = ES.enter_context(nc.sbuf_tensor(_u("gb0"), [128, 4096], BF16))
        gb1 = ES.enter_context(nc.sbuf_tensor(_u("gb1"), [128, 4096], BF16))
        wf = ES.enter_context(nc.sbuf_tensor(_u("wf"), [128, 4096], F32))
        junk = ES.enter_context(nc.sbuf_tensor(_u("junk"), [128, 4096], BF16))
        og = ES.enter_context(nc.sbuf_tensor(_u("og"), [128, 8], I32))
        oh = ES.enter_context(nc.sbuf_tensor(_u("oh"), [128, 8, 8], I32))
        yb = ES.enter_context(nc.sbuf_tensor(_u("yb"), [128, 16], F32))
        sl = ES.enter_context(nc.sbuf_tensor(_u("sl"), [128, 16], F32))
        af = ES.enter_context(nc.sbuf_tensor(_u("af"), [128, 16], F32))
        ix = ES.enter_context(nc.sbuf_tensor(_u("ix"), [128, 16], I32))
        sm = ES.enter_context(nc.sbuf_tensor(_u("sm"), [128, 8], F32))
        gb = [gb0, gb1]
        P.dma("sp", lambda q: q.dma_start(out=wf[:], in_=D["wfbc"]), w=["wf"])
        P.dma("sp", lambda q: q.dma_start(out=og[:], in_=D["own_g"]), w=["og"])
        P.dma("sp", lambda q: q.dma_start(out=oh[:], in_=D["own_h"]), w=["oh"])
        P.dma("sp", lambda q: q.dma_start(out=yb[:], in_=D["ybase"]), w=["yb"])
        for k in range(8):
            P.dma("pool", lambda q: q.indirect_dma_start(out=sl[:], out_offset=None, in_=D["slot_tab"], in_offset=bass.IndirectOffsetOnAxis(ap=og[:, k:k + 1], axis=0),
                                                         bounds_check=BREG(nc, NR - 1), oob_is_err=False), r=["og", "slot_tab"], w=["sl"])
            P.dma("pool", lambda q: q.indirect_dma_start(out=af[:], out_offset=None, in_=D["aff_tab"], in_offset=bass.IndirectOffsetOnAxis(ap=og[:, k:k + 1], axis=0),
                                                         bounds_check=BREG(nc, NR - 1), oob_is_err=False), r=["og", "aff_tab"], w=["af"])
            P.op("dve", lambda e: e.tensor_tensor(out=sl[:], in0=sl[:], in1=yb[:], op=ALU.add), r=["sl", "yb"], w=["sl"])
            P.op("dve", lambda e: e.tensor_copy(out=ix[:], in_=sl[:]), r=["sl"], w=["ix"])
            for r in range(8):
                P.dma("pool", lambda q: q.indirect_dma_start(out=acc[:, r * 512:(r + 1) * 512], out_offset=None, in_=D["h2_all"],
                                                             in_offset=bass.IndirectOffsetOnAxis(ap=oh[:, k, r:r + 1], axis=0),
                                                             bounds_check=BREG(nc, 8 * NR - 1), oob_is_err=False), r=["oh", "h2_all"], w=["acc"])
            for ex in range(16):
                b = ex % 2
                P.op("pool", lambda e: e.memset(gb[b][:], 0.0), w=[("gb", b)])
                P.dma("pool", lambda q: q.indirect_dma_start(out=gb[b][:], out_offset=None, in_=D["ye_all"], in_offset=bass.IndirectOffsetOnAxis(ap=ix[:, ex:ex + 1], axis=0),
                                                             bounds_check=BREG(nc, 8 * 2 * NS - 1), oob_is_err=False), r=["ix", "ye_all", ("gb", b)], w=[("gb", b)])
                P.op("dve", lambda e: e.scalar_tensor_tensor(out=acc[:], in0=gb[b][:], scalar=af[:, ex:ex + 1], in1=acc[:], op0=ALU.mult, op1=ALU.add),
                     r=[("gb", b), "af", "acc"], w=["acc"])
            P.op("act", lambda e: e.activation(out=junk[:], in_=acc[:], func=AF.Square, accum_out=sm[:, 0:1]), r=["acc"], w=["junk", "ss"])
            P.op("act", lambda e: e.activation(out=sm[:, 1:2], in_=sm[:, 0:1], func=AF.Sqrt, bias=EPS, scale=1.0 / 4096), r=["ss"], w=["rt"])
            P.op("dve", lambda e: e.reciprocal(out=sm[:, 2:3], in_=sm[:, 1:2]), r=["rt"], w=["rstd"])
            P.op("dve", lambda e: e.scalar_tensor_tensor(out=acc[:], in0=acc[:], scalar=sm[:, 2:3], in1=wf[:], op0=ALU.mult, op1=ALU.mult), r=["acc", "rstd", "wf"], w=["acc"])
            P.dma("sp", lambda q: q.dma_start(out=D["out"][k * 128:(k + 1) * 128, :], in_=acc[:]), r=["acc"], w=["out"])
import os
import ml_dtypes
from concourse.bass_utils import run_bass_kernel_spmd

BF = ml_dtypes.bfloat16
PHASES = ["A", "DNF", "DNB", "SWA", "AG1", "B1", "AG2", "B2", "AG3", "B3", "TOPK", "C", "AG4", "D"]


def build(stop_after="D", dbg=(), nt=NT, lite=False):
    nc = bass.Bass("TRN2", target_bir_lowering=False)
    D = {}

    INS = []

    def inp(name, shape, dt):
        if lite and name in ("wga", "wgb", "wa", "wb", "wo", "xcol", "wg", "wu", "wd", "w2bc", "wfbc", "wr"):
            shape = [1] * (len(shape) - 1) + [16]
        INS.append((name, list(shape), dt))
        D[name] = nc.dram_tensor(name, list(shape), dt, kind="ExternalInput").ap()

    def scr(name, shape, dt):
        if name.endswith("_all") and name not in dbg:
            D[name] = nc.dram_tensor(name, list(shape), dt, addr_space="Shared").ap()
            return
        if name in dbg:
            D[name] = nc.dram_tensor(name, list(shape), dt, kind="ExternalOutput").ap()
        else:
            D[name] = nc.dram_tensor(name, list(shape), dt).ap()

    inp("xp", [NR, 4096], F32); inp("w_in", [4096, CW], F32); inp("w1c", [128, 32], F32); inp("dnp", [128, 8], F32)
    inp("cwbc", [128, 3, 768], F32); inp("shm", [128, 5, 128], BF16); inp("dnmask", [2, 128, 3, 128], F32)
    inp("lastsel", [2, 128, 4], F32); inp("onwbc", [128, 128], F32); inp("swbias", [128, 3, 2, 400], F32)
    inp("swmbias", [16, 144], F32); inp("sinkbc", [128, 2], F32)
    inp("wga", [4096, 512], F32); inp("wgb", [4096, 512], F32); inp("wa", [2048, 512], F32); inp("wb", [2048, 512], F32)
    inp("wo", [4096, 512], F32); inp("xcol", [NR, 512], F32); inp("w2bc", [128, 4096], F32); inp("wr", [4096, 16], F32)
    inp("validbc", [128, NT, 16], F32); inp("lstrict", [128, 128], F32)
    inp("wg", [2, 4096, 2048], F32); inp("wu", [2, 4096, 2048], F32); inp("wd", [2, 2048, 4096], F32)
    inp("wfbc", [128, 4096], F32); inp("own_g", [128, 8], I32); inp("own_h", [128, 8, 8], I32); inp("ybase", [128, 16], F32)
    inp("identb_d", [128, 128], BF16); inp("identf_d", [128, 128], F32)
    D["out"] = nc.dram_tensor("out", [1024, 4096], F32, kind="ExternalOutput").ap()
    scr("dn_qkv", [NR, 768], BF16); scr("dn_z", [NR, 256], BF16); scr("dn_sc", [NR, 8], F32); scr("sw_qkv", [NR, 512], BF16)
    scr("o_f", [NR, 256], F32); scr("xT_s", [NR, 4096], BF16); scr("oab", [NR, 512], BF16); scr("oab_all", [8 * NR, 512], BF16)
    scr("mixed", [NR, 512], BF16); scr("mixed_all", [8 * NR, 512], BF16); scr("h2c", [NR, 512], F32); scr("h2_all", [8 * NR, 512], F32)
    scr("hn2", [NR, 4096], BF16); scr("slot_tab", [NR, 16], F32); scr("aff_tab", [NR, 16], F32)
    scr("Xe0", [NS, 4096], BF16); scr("Xe1", [NS, 4096], BF16); scr("ye", [2 * NS, 4096], BF16); scr("ye_all", [16 * NS, 4096], BF16)
    P = Prog(nc)
    last = PHASES.index(stop_after)

    def on(ph):
        return PHASES.index(ph) <= last

    with ExitStack() as ES:
        identb = ES.enter_context(nc.sbuf_tensor(_u("identb"), [128, 128], BF16))
        identf = ES.enter_context(nc.sbuf_tensor(_u("identf"), [128, 128], F32))
        onesf = ES.enter_context(nc.sbuf_tensor(_u("onesf"), [128, 128], F32))
        affs = ES.enter_context(nc.sbuf_tensor(_u("affs"), [128, NT, 16], F32))
        sloti = ES.enter_context(nc.sbuf_tensor(_u("sloti"), [128, NT, 16], I32))
        C = {"identb": identb, "identf": identf, "onesf": onesf}
        P.dma("sp", lambda q: q.dma_start(out=identb[:], in_=D["identb_d"]), w=["identb"])
        P.dma("sp", lambda q: q.dma_start(out=identf[:], in_=D["identf_d"]), w=["identf"])
        P.op("pool", lambda e: e.memset(onesf[:], 1.0), w=["onesf"])
        P.barrier()
        if on("A"):
            phase_A(P, nc, D, C, nt)
        P.barrier()
        if on("DNF"):
            dn_pass(P, nc, D, C, 0, nt)
        P.barrier()
        if on("DNB"):
            dn_pass(P, nc, D, C, 1, nt)
        P.barrier()
        if on("SWA"):
            swa_pass(P, nc, D, C, nt)
        P.barrier()
        if on("AG1"):
            allgather(P, D["oab"], D["oab_all"], ["oab"], ["oab_all"])
        P.barrier()
        if on("B1"):
            phase_B1(P, nc, D, C, nt)
        P.barrier()
        if on("AG2"):
            allgather(P, D["mixed"], D["mixed_all"], ["mixed"], ["mixed_all"])
        P.barrier()
        if on("B2"):
            phase_B2(P, nc, D, C, nt)
        P.barrier()
        if on("AG3"):
            allgather(P, D["h2c"], D["h2_all"], ["h2c"], ["h2_all"])
        P.barrier()
        if on("B3"):
            phase_B3(P, nc, D, C, affs, nt)
        P.barrier()
        if on("TOPK"):
            phase_topk(P, nc, D, C, affs, sloti, nt)
        P.barrier()
        if on("C"):
            phase_C(P, nc, D, C, sloti, nt)
        P.barrier()
        if on("AG4"):
            allgather(P, D["ye"], D["ye_all"], ["ye"], ["ye_all"])
        P.barrier()
        if on("D"):
            phase_D(P, nc, D, C)
        keys = ["out"] + list(dbg)
        P.wait_all("sp", keys)
        P.wait_all("pool", keys)
    P.INS = INS
    return nc, P


def host_inputs(x, meta_tokens, norm1_w, w_in, conv_w, a_log_fwd, a_log_bwd, dt_bias_fwd, dt_bias_bwd,
                out_norm_w, w_branch_a, attn_sink, w_branch_b, w_out, norm2_w, w_router, w_gate, w_up,
                w_down, norm_f_w, cores=range(8)):
    f32 = np.float32
    x = np.asarray(x, f32)
    xp = np.concatenate([np.zeros((112, 4096), f32), np.asarray(meta_tokens, f32), x[0]], 0)
    w_in = np.asarray(w_in, f32)[0]
    conv_w = np.asarray(conv_w, f32)[0]
    rep = lambda v, n=128: np.ascontiguousarray(np.broadcast_to(np.asarray(v, f32)[None], (n,) + np.asarray(v).shape))
    ar = np.arange(128)
    ch = ar // 64
    same = ch[:, None] == ch[None, :]
    lt = ar[:, None] < ar[None, :]
    le = ar[:, None] <= ar[None, :]
    dnmask = np.zeros((2, 128, 3, 128), f32)
    dnmask[0, :, 0, :] = same & le
    dnmask[0, :, 1, :] = same & lt.T
    dnmask[0, :, 2, :] = same & le
    dnmask[1, :, 0, :] = same & le.T
    dnmask[1, :, 1, :] = same & lt
    dnmask[1, :, 2, :] = same & le.T
    lastsel = np.zeros((2, 128, 4), f32)
    lastsel[:, 0:64, 2] = 1; lastsel[:, 64:128, 3] = 1
    lastsel[0, 63, 0] = 1; lastsel[0, 127, 1] = 1; lastsel[1, 0, 0] = 1; lastsel[1, 64, 1] = 1
    shm = np.zeros((128, 5, 128), f32)
    for t in range(1, 128):
        shm[t - 1, 0, t] = 1
        shm[t, 1, t - 1] = 1
    shm[127, 2, 0] = 1
    shm[0, 3, 127] = 1
    shm = shm.astype(BF)
    q_i = np.arange(128)[:, None]
    kk = np.arange(384)[None, :]
    dist = 128 + q_i - kk
    okb = np.abs(dist) <= 128
    slopes = 2.0 ** (-8.0 * np.arange(1, 17, dtype=np.float64) / 16)
    mb = np.zeros((16, 144), f32)
    jj = np.arange(128)[None, :]
    ii = np.arange(16)[:, None]
    mb[:, 0:128] = np.where(jj <= 112 + ii, 0.0, -30000.0)
    validbc = np.ones((128, NT, 16), f32)
    validbc[0:112, 0, :] = 0
    lstrict = (ar[:, None] < ar[None, :]).astype(f32)
    common = dict(xp=xp, shm=shm, dnmask=dnmask, lastsel=lastsel, onwbc=rep(np.asarray(out_norm_w, f32)[0]),
                  swmbias=mb, w1c=np.ascontiguousarray(np.asarray(norm1_w, f32)[0].reshape(32, 128).T),
                  w2bc=rep(np.asarray(norm2_w, f32)[0]), wfbc=rep(np.asarray(norm_f_w, f32)), validbc=validbc, lstrict=lstrict,
                  identb_d=np.eye(128, dtype=f32).astype(BF), identf_d=np.eye(128, dtype=f32))
    wa_full = np.asarray(w_branch_a, f32)[0]; wb_full = np.asarray(w_branch_b, f32)[0]; wo_full = np.asarray(w_out, f32)[0]
    wr_full = np.asarray(w_router, f32)[0]
    alf = np.asarray(a_log_fwd, f32)[0]; alb = np.asarray(a_log_bwd, f32)[0]
    dbf = np.asarray(dt_bias_fwd, f32)[0]; dbb = np.asarray(dt_bias_bwd, f32)[0]
    sink = np.asarray(attn_sink, f32)[0]
    maps = []
    for c in cores:
        hs = [2 * c, 2 * c + 1]
        kv = c // 2
        cols = []
        for base in (0, 2048, 4096, 6144):
            for h in hs:
                cols.append(np.arange(base + h * 128, base + (h + 1) * 128))
        for h in hs:
            cols.append(np.arange(8256 + h * 128, 8256 + (h + 1) * 128))
        cols.append(np.arange(10304 + kv * 128, 10304 + (kv + 1) * 128))
        cols.append(np.arange(10816 + kv * 128, 10816 + (kv + 1) * 128))
        for base in (8192, 8208, 8224, 8240):
            cols.append(np.array([base + hs[0], base + hs[1]]))
        cols = np.concatenate(cols)
        assert cols.shape[0] == CW
        ccols = np.concatenate([np.arange(base + h * 128, base + (h + 1) * 128) for base in (0, 2048, 4096) for h in hs])
        bias = np.zeros((128, 3, 2, 400), f32)
        for hl, h in enumerate(hs):
            for var in range(3):
                ok = okb.copy()
                if var == 0:
                    ok = ok & (kk >= 128)
                if var == 2:
                    ok = ok & (kk < 256)
                bias[:, var, hl, 0:384] = np.where(ok, -slopes[h] * np.abs(dist), -30000.0)
        perm = hs + [e for e in range(16) if e not in hs]
        og = ((1 + 8 * c + np.arange(8))[None, :] * 128 + ar[:, None]).astype(np.int32)
        oh = (np.arange(8)[None, None, :] * NR + og[:, :, None]).astype(np.int32)
        yb = np.array([(e // 2) * 2 * NS + (e % 2) * NS for e in perm], f32)
        m = dict(common)
        m.update(w_in=np.ascontiguousarray(w_in[:, cols]),
                 dnp=rep(np.array([alf[hs[0]], alf[hs[1]], alb[hs[0]], alb[hs[1]], dbf[hs[0]], dbf[hs[1]], dbb[hs[0]], dbb[hs[1]]], f32)),
                 cwbc=rep(np.ascontiguousarray(conv_w[:, ccols])), swbias=bias, sinkbc=rep(np.array([sink[hs[0]], sink[hs[1]]], f32)),
                 wga=np.ascontiguousarray(w_in[:, 11328 + 512 * c:11328 + 512 * (c + 1)]),
                 wgb=np.ascontiguousarray(w_in[:, 15424 + 512 * c:15424 + 512 * (c + 1)]),
                 wa=np.ascontiguousarray(wa_full[:, 512 * c:512 * (c + 1)]), wb=np.ascontiguousarray(wb_full[:, 512 * c:512 * (c + 1)]),
                 wo=np.ascontiguousarray(wo_full[:, 512 * c:512 * (c + 1)]), xcol=np.ascontiguousarray(xp[:, 512 * c:512 * (c + 1)]),
                 wr=np.ascontiguousarray(wr_full[:, perm]),
                 wg=np.ascontiguousarray(np.asarray(w_gate)[0, 2 * c:2 * c + 2], dtype=f32), wu=np.ascontiguousarray(np.asarray(w_up)[0, 2 * c:2 * c + 2], dtype=f32),
                 wd=np.ascontiguousarray(np.asarray(w_down)[0, 2 * c:2 * c + 2], dtype=f32),
                 own_g=og, own_h=oh, ybase=rep(yb))
        maps.append(m)
    return maps


def kernel(**inputs):
    maps = host_inputs(**inputs)
    nc, _ = build()
    res = run_bass_kernel_spmd(nc, maps, core_ids=list(range(8)))
    out = np.concatenate([np.asarray(res.results[c]["out"], np.float32) for c in range(8)], 0)
    return out.reshape(1, 8192, 4096)
```
